# Optimizing a Trainium2 kernel written in Bass

```python
import jax, jax.numpy as jnp
from jax import lax
import numpy as np

D_MODEL = 2048
BATCH = 16
SEQ = 2048
DEPTH = 2

GRID_W = 64
CTX_LEN = 256
EPS = 1e-6
ROPE_THETA = 10000.0
BLOCK = 128
NEG_INF = -1e30
MLA_HEADS = 8
MLA_Q_RANK = 512
MLA_KV_RANK = 256
MLA_NOPE = 128
MLA_ROPE = 64
MLA_V = 128
MLA_IN = MLA_Q_RANK + MLA_KV_RANK + MLA_ROPE
CONV_CH = 1024
CONV_WIDTH = 31
EVEN_IN = MLA_IN + 2 * CONV_CH
EVEN_MIX = MLA_HEADS * MLA_V + CONV_CH
HEAD_DIM = 128
WIN_Q_HEADS = 12
WIN_KV_HEADS = 4
WINDOW = 128
FNET_GROUPS = 4
FNET_CH = 128
ODD_Q = WIN_Q_HEADS * HEAD_DIM
ODD_KV = WIN_KV_HEADS * HEAD_DIM
ODD_IN = ODD_Q + 2 * ODD_KV + FNET_GROUPS * FNET_CH
ODD_MIX = ODD_Q + FNET_GROUPS * FNET_CH
DENSE_FF = 5632
N_EXPERTS = 8
TOP_K = 2
EXPERT_FF = 7168

kernel_name = "hybrid_mla_conv_swa_fnet_moe_dit"


def _rmsnorm(x, g):
    xf = x.astype(jnp.float32)
    y = xf * lax.rsqrt(jnp.mean(xf * xf, axis=-1, keepdims=True) + EPS)
    return (y * g.astype(jnp.float32)).astype(x.dtype)


def _layernorm(x, g, b):
    xf = x.astype(jnp.float32)
    mu = jnp.mean(xf, axis=-1, keepdims=True)
    xc = xf - mu
    y = xc * lax.rsqrt(jnp.mean(xc * xc, axis=-1, keepdims=True) + EPS)
    return (y * g.astype(jnp.float32) + b.astype(jnp.float32)).astype(x.dtype)


def _modulation(cvec, w, b):
    return jnp.split(jax.nn.silu(cvec) @ w + b, 6, axis=-1)


def _modulate(h, shift, scale):
    return h * (1.0 + scale) + shift


def _axial_rope_tables(rows, rot_dim):
    row = jnp.repeat(jnp.arange(rows, dtype=jnp.float32), GRID_W)
    col = jnp.tile(jnp.arange(GRID_W, dtype=jnp.float32), rows)
    half = rot_dim // 2
    inv = ROPE_THETA ** (-jnp.arange(0, half, 2, dtype=jnp.float32) / half)
    ang_r = row[:, None] * inv[None, :]
    ang_c = col[:, None] * inv[None, :]
    return (jnp.cos(ang_r), jnp.sin(ang_r), jnp.cos(ang_c), jnp.sin(ang_c))


def _rope_rotate(x, cos, sin):
    x1, x2 = jnp.split(x, 2, axis=-1)
    cos = cos[None, :, None, :]
    sin = sin[None, :, None, :]
    return jnp.concatenate([x1 * cos - x2 * sin, x1 * sin + x2 * cos], axis=-1)


def _apply_axial_rope(x, tables):
    cos_r, sin_r, cos_c, sin_c = tables
    x_row, x_col = jnp.split(x.astype(jnp.float32), 2, axis=-1)
    y = jnp.concatenate([_rope_rotate(x_row, cos_r, sin_r), _rope_rotate(x_col, cos_c, sin_c)], axis=-1)
    return y.astype(x.dtype)


def _rope_tail(x, rope):
    if rope is None:
        return x
    return jnp.concatenate([x[..., :MLA_NOPE], _apply_axial_rope(x[..., MLA_NOPE:], rope)], axis=-1)


def _dense_block_attention(q, k, v, scale):
    B, T, H, dk = q.shape
    qb = q.reshape(B, T // BLOCK, BLOCK, H, dk).transpose(1, 0, 2, 3, 4)

    def one_block(q_blk):
        s = jnp.einsum("bqhd,bkhd->bhqk", q_blk, k, preferred_element_type=jnp.float32) * scale
        p = jax.nn.softmax(s, axis=-1).astype(v.dtype)
        return jnp.einsum("bhqk,bkhd->bqhd", p, v)

    out = lax.map(one_block, qb)
    return out.transpose(1, 0, 2, 3, 4).reshape(B, T, -1)


def _sink_softmax(s, sink):
    m = jnp.maximum(jnp.max(s, axis=-1, keepdims=True), sink)
    e = jnp.exp(s - m)
    return e / (jnp.sum(e, axis=-1, keepdims=True) + jnp.exp(sink - m))


def _window_sink_attention(q, k, v, k_ctx, v_ctx, sink, scale):
    B, S, H, d = q.shape
    n_kv = k.shape[2]
    g = H // n_kv
    span = BLOCK + 2 * WINDOW
    pad = ((0, 0), (WINDOW, WINDOW), (0, 0), (0, 0))
    k_pad = jnp.pad(k, pad)
    v_pad = jnp.pad(v, pad)
    qg = q.reshape(B, S, n_kv, g, d)
    sink_g = sink.astype(jnp.float32).reshape(1, n_kv, g, 1, 1)
    offs_q = jnp.arange(BLOCK)
    offs_k = jnp.arange(span)

    def one_block(blk):
        start = blk * BLOCK
        qb = lax.dynamic_slice_in_dim(qg, start, BLOCK, axis=1)
        kb = lax.dynamic_slice_in_dim(k_pad, start, span, axis=1)
        vb = lax.dynamic_slice_in_dim(v_pad, start, span, axis=1)
        q_pos = start + offs_q
        k_pos = start - WINDOW + offs_k
        valid = ((jnp.abs(q_pos[:, None] - k_pos[None, :]) <= WINDOW)
                 & (k_pos >= 0)[None, :] & (k_pos < S)[None, :])
        s_win = jnp.einsum("bqngd,btnd->bngqt", qb, kb, preferred_element_type=jnp.float32) * scale
        s_win = jnp.where(valid, s_win, NEG_INF)
        s_ctx = jnp.einsum("bqngd,bcnd->bngqc", qb, k_ctx, preferred_element_type=jnp.float32) * scale
        p = _sink_softmax(jnp.concatenate([s_win, s_ctx], axis=-1), sink_g).astype(v.dtype)
        o = (jnp.einsum("bngqt,btnd->bqngd", p[..., :span], vb)
             + jnp.einsum("bngqc,bcnd->bqngd", p[..., span:], v_ctx))
        return o.reshape(B, BLOCK, H * d)

    out = lax.map(one_block, jnp.arange(S // BLOCK))
    return out.transpose(1, 0, 2, 3).reshape(B, S, H * d)


def _full_sink_attention(q, k, v, sink, scale):
    B, T, H, d = q.shape
    n_kv = k.shape[2]
    g = H // n_kv
    qg = q.reshape(B, T, n_kv, g, d)
    s = jnp.einsum("bqngd,bcnd->bngqc", qg, k, preferred_element_type=jnp.float32) * scale
    p = _sink_softmax(s, sink.astype(jnp.float32).reshape(1, n_kv, g, 1, 1)).astype(v.dtype)
    return jnp.einsum("bngqc,bcnd->bqngd", p, v).reshape(B, T, H * d)


def _mla_queries(p, q_a_g, w_q_b, q_norm_g, rope):
    B, T, _ = p.shape
    q = (_rmsnorm(p[..., :MLA_Q_RANK], q_a_g) @ w_q_b).reshape(B, T, MLA_HEADS, MLA_NOPE + MLA_ROPE)
    return _rope_tail(_rmsnorm(q, q_norm_g), rope)


def _mla_keys_values(p, kv_a_g, w_kv_b, k_norm_g, rope):
    B, T, _ = p.shape
    kv_a = p[..., MLA_Q_RANK:MLA_Q_RANK + MLA_KV_RANK]
    k_pe = p[..., MLA_Q_RANK + MLA_KV_RANK:MLA_IN]
    kv = (_rmsnorm(kv_a, kv_a_g) @ w_kv_b).reshape(B, T, MLA_HEADS, MLA_NOPE + MLA_V)
    k_nope, v = kv[..., :MLA_NOPE], kv[..., MLA_NOPE:]
    k_pe = jnp.broadcast_to(k_pe[:, :, None, :], (B, T, MLA_HEADS, MLA_ROPE))
    k = _rmsnorm(jnp.concatenate([k_nope, k_pe], axis=-1), k_norm_g)
    return _rope_tail(k, rope), v


def _conformer_conv(u, dw_w, dw_b, ln_g, ln_b):
    a, gate = jnp.split(u, 2, axis=-1)
    h = a * jax.nn.sigmoid(gate)
    h = lax.conv_general_dilated(h, dw_w[:, None, :], window_strides=(1,),
                                 padding=[(CONV_WIDTH // 2, CONV_WIDTH // 2)],
                                 dimension_numbers=("NWC", "WIO", "NWC"),
                                 feature_group_count=CONV_CH) + dw_b
    return jax.nn.silu(_layernorm(h, ln_g, ln_b))


def _fourier(f):
    B, T, _ = f.shape
    fg = f.reshape(B, T, FNET_GROUPS, FNET_CH).astype(jnp.float32)
    y = jnp.fft.fft2(fg, axes=(1, 3), norm="ortho").real
    return y.reshape(B, T, FNET_GROUPS * FNET_CH).astype(f.dtype)


def _even_mixer(u_lat, u_ctx, need_ctx, rope, w_in, q_a_g, w_q_b, kv_a_g, w_kv_b, q_norm_g, k_norm_g,
                dw_w, dw_b, ln_g, ln_b, w_out):
    p_lat = u_lat @ w_in
    p_ctx = u_ctx @ w_in
    scale = (MLA_NOPE + MLA_ROPE) ** -0.5
    q_l = _mla_queries(p_lat, q_a_g, w_q_b, q_norm_g, rope)
    k_l, v_l = _mla_keys_values(p_lat, kv_a_g, w_kv_b, k_norm_g, rope)
    k_c, v_c = _mla_keys_values(p_ctx, kv_a_g, w_kv_b, k_norm_g, None)
    att_l = _dense_block_attention(q_l, jnp.concatenate([k_l, k_c], axis=1),
                                   jnp.concatenate([v_l, v_c], axis=1), scale)
    conv_l = _conformer_conv(p_lat[..., MLA_IN:], dw_w, dw_b, ln_g, ln_b)
    out_l = jnp.concatenate([att_l, conv_l], axis=-1) @ w_out
    if not need_ctx:
        return out_l, None
    q_c = _mla_queries(p_ctx, q_a_g, w_q_b, q_norm_g, None)
    att_c = _dense_block_attention(q_c, k_c, v_c, scale)
    conv_c = _conformer_conv(p_ctx[..., MLA_IN:], dw_w, dw_b, ln_g, ln_b)
    out_c = jnp.concatenate([att_c, conv_c], axis=-1) @ w_out
    return out_l, out_c


def _odd_mixer(u_lat, u_ctx, need_ctx, rope, w_in, q_norm_g, k_norm_g, sink, w_out):
    B, S, _ = u_lat.shape
    L = u_ctx.shape[1]
    scale = HEAD_DIM ** -0.5
    p = u_lat @ w_in
    q = _apply_axial_rope(_rmsnorm(p[..., :ODD_Q].reshape(B, S, WIN_Q_HEADS, HEAD_DIM), q_norm_g), rope)
    k = _apply_axial_rope(_rmsnorm(p[..., ODD_Q:ODD_Q + ODD_KV].reshape(B, S, WIN_KV_HEADS, HEAD_DIM), k_norm_g), rope)
    v = p[..., ODD_Q + ODD_KV:ODD_Q + 2 * ODD_KV].reshape(B, S, WIN_KV_HEADS, HEAD_DIM)
    f = p[..., ODD_Q + 2 * ODD_KV:]
    p_c = u_ctx @ (w_in if need_ctx else w_in[:, ODD_Q:ODD_Q + 2 * ODD_KV])
    off = ODD_Q if need_ctx else 0
    k_c = _rmsnorm(p_c[..., off:off + ODD_KV].reshape(B, L, WIN_KV_HEADS, HEAD_DIM), k_norm_g)
    v_c = p_c[..., off + ODD_KV:off + 2 * ODD_KV].reshape(B, L, WIN_KV_HEADS, HEAD_DIM)
    att = _window_sink_attention(q, k, v, k_c, v_c, sink, scale)
    out_l = jnp.concatenate([att, _fourier(f)], axis=-1) @ w_out
    if not need_ctx:
        return out_l, None
    q_c = _rmsnorm(p_c[..., :ODD_Q].reshape(B, L, WIN_Q_HEADS, HEAD_DIM), q_norm_g)
    att_c = _full_sink_attention(q_c, k_c, v_c, sink, scale)
    out_c = jnp.concatenate([att_c, _fourier(p_c[..., ODD_Q + 2 * ODD_KV:])], axis=-1) @ w_out
    return out_l, out_c


def _swiglu(h, w_gate, w_up, w_down):
    return (jax.nn.silu(h @ w_gate) * (h @ w_up)) @ w_down


def _moe_swiglu(h, router_w, w_gate, w_up, w_down):
    logits = (h @ router_w).astype(jnp.float32)
    top_v, top_i = lax.top_k(logits, TOP_K)
    gates = jax.nn.softmax(top_v, axis=-1)
    flat_e = top_i.reshape(-1)
    order = jnp.argsort(flat_e)
    tok = order // TOP_K
    xs = jnp.take(h, tok, axis=0)
    sizes = jnp.bincount(flat_e, length=N_EXPERTS).astype(jnp.int32)
    hid = jax.nn.silu(lax.ragged_dot(xs, w_gate, sizes)) * lax.ragged_dot(xs, w_up, sizes)
    ys = lax.ragged_dot(hid, w_down, sizes)
    ys = ys * gates.reshape(-1)[order][:, None].astype(ys.dtype)
    return jnp.zeros_like(h).at[tok].add(ys.astype(h.dtype))


def setup_inputs(seed: int = 0) -> dict:
    key = jax.random.key(seed)
    keys = jax.random.split(key, 40)
    counter = [0]
    n_even = (DEPTH + 1) // 2
    n_odd = DEPTH // 2
    D = D_MODEL

    def nrm(shape, scale):
        k = keys[counter[0]]
        counter[0] += 1
        return jax.random.normal(k, shape, jnp.float32) * scale

    def gain(shape):
        return 1.0 + nrm(shape, 0.02)

    return {
        "x": nrm((BATCH, SEQ, D), 1.0),
        "c": nrm((BATCH, D), 1.0),
        "ctx": nrm((BATCH, CTX_LEN, D), 1.0),
        "c_ctx": nrm((D,), 1.0),
        "ada_w": nrm((DEPTH, D, 6 * D), 0.5 * D ** -0.5),
        "ada_b": nrm((DEPTH, 6 * D), 0.01),
        "mix_norm_g": gain((DEPTH, D)),
        "ffn_norm_g": gain((DEPTH, D)),
        "even_w_in": nrm((n_even, D, EVEN_IN), D ** -0.5),
        "mla_q_a_norm_g": gain((n_even, MLA_Q_RANK)),
        "mla_w_q_b": nrm((n_even, MLA_Q_RANK, MLA_HEADS * (MLA_NOPE + MLA_ROPE)), MLA_Q_RANK ** -0.5),
        "mla_kv_a_norm_g": gain((n_even, MLA_KV_RANK)),
        "mla_w_kv_b": nrm((n_even, MLA_KV_RANK, MLA_HEADS * (MLA_NOPE + MLA_V)), MLA_KV_RANK ** -0.5),
        "mla_q_norm_g": gain((n_even, MLA_NOPE + MLA_ROPE)),
        "mla_k_norm_g": gain((n_even, MLA_NOPE + MLA_ROPE)),
        "conv_dw_w": nrm((n_even, CONV_WIDTH, CONV_CH), CONV_WIDTH ** -0.5),
        "conv_dw_b": nrm((n_even, CONV_CH), 0.01),
        "conv_ln_g": gain((n_even, CONV_CH)),
        "conv_ln_b": nrm((n_even, CONV_CH), 0.01),
        "even_w_out": nrm((n_even, EVEN_MIX, D), EVEN_MIX ** -0.5),
        "dense_w_gate": nrm((n_even, D, DENSE_FF), D ** -0.5),
        "dense_w_up": nrm((n_even, D, DENSE_FF), D ** -0.5),
        "dense_w_down": nrm((n_even, DENSE_FF, D), DENSE_FF ** -0.5),
        "odd_w_in": nrm((n_odd, D, ODD_IN), D ** -0.5),
        "swa_q_norm_g": gain((n_odd, HEAD_DIM)),
        "swa_k_norm_g": gain((n_odd, HEAD_DIM)),
        "swa_sink": nrm((n_odd, WIN_Q_HEADS), 1.0),
        "odd_w_out": nrm((n_odd, ODD_MIX, D), ODD_MIX ** -0.5),
        "router_w": nrm((n_odd, D, N_EXPERTS), D ** -0.5),
        "expert_w_gate": nrm((n_odd, N_EXPERTS, D, EXPERT_FF), D ** -0.5),
        "expert_w_up": nrm((n_odd, N_EXPERTS, D, EXPERT_FF), D ** -0.5),
        "expert_w_down": nrm((n_odd, N_EXPERTS, EXPERT_FF, D), EXPERT_FF ** -0.5),
    }


def reference(x, c, ctx, c_ctx, ada_w, ada_b, mix_norm_g, ffn_norm_g,
              even_w_in, mla_q_a_norm_g, mla_w_q_b, mla_kv_a_norm_g, mla_w_kv_b, mla_q_norm_g, mla_k_norm_g,
              conv_dw_w, conv_dw_b, conv_ln_g, conv_ln_b, even_w_out,
              dense_w_gate, dense_w_up, dense_w_down,
              odd_w_in, swa_q_norm_g, swa_k_norm_g, swa_sink, odd_w_out,
              router_w, expert_w_gate, expert_w_up, expert_w_down):
    B, S, D = x.shape
    rows = S // GRID_W
    rope_mla = _axial_rope_tables(rows, MLA_ROPE)
    rope_swa = _axial_rope_tables(rows, HEAD_DIM)
    h_ctx = ctx
    for i in range(DEPTH):
        j = i // 2
        last = i == DEPTH - 1
        sm, cm, gm, sf, cf, gf = [m[:, None, :] for m in _modulation(c, ada_w[i], ada_b[i])]
        xsm, xcm, xgm, xsf, xcf, xgf = _modulation(c_ctx, ada_w[i], ada_b[i])
        u_lat = _modulate(_rmsnorm(x, mix_norm_g[i]), sm, cm)
        u_ctx = _modulate(_rmsnorm(h_ctx, mix_norm_g[i]), xsm, xcm)
        if i % 2 == 0:
            m_lat, m_ctx = _even_mixer(u_lat, u_ctx, not last, rope_mla, even_w_in[j],
                                       mla_q_a_norm_g[j], mla_w_q_b[j], mla_kv_a_norm_g[j], mla_w_kv_b[j],
                                       mla_q_norm_g[j], mla_k_norm_g[j], conv_dw_w[j], conv_dw_b[j],
                                       conv_ln_g[j], conv_ln_b[j], even_w_out[j])
        else:
            m_lat, m_ctx = _odd_mixer(u_lat, u_ctx, not last, rope_swa, odd_w_in[j],
                                      swa_q_norm_g[j], swa_k_norm_g[j], swa_sink[j], odd_w_out[j])
        x = x + gm * m_lat
        f_in = _modulate(_rmsnorm(x, ffn_norm_g[i]), sf, cf).reshape(B * S, D)
        if not last:
            h_ctx = h_ctx + xgm * m_ctx
            f_ctx = _modulate(_rmsnorm(h_ctx, ffn_norm_g[i]), xsf, xcf)
            f_in = jnp.concatenate([f_in, f_ctx.reshape(-1, D)], axis=0)
        if i % 2 == 0:
            f_out = _swiglu(f_in, dense_w_gate[j], dense_w_up[j], dense_w_down[j])
        else:
            f_out = _moe_swiglu(f_in, router_w[j], expert_w_gate[j], expert_w_up[j], expert_w_down[j])
        x = x + gf * f_out[:B * S].reshape(B, S, D)
        if not last:
            h_ctx = h_ctx + xgf * f_out[B * S:].reshape(h_ctx.shape)
    return x
```

```python
import numpy as np
import concourse.bass as bass
import concourse.mybir as mybir

F32 = mybir.dt.float32
BF16 = mybir.dt.bfloat16
AF = mybir.ActivationFunctionType
ALU = mybir.AluOpType
DT_SIZE = {F32: 4, BF16: 2}


class Prog:
    CE = ("pe", "act", "dve", "pool")
    DQ = ("sp", "pool")

    def __init__(self, nc, nslots=14):
        self.nc = nc
        self.ops = []
        self.csem = {e: nc.alloc_semaphore("c_" + e) for e in self.CE}
        self.ccount = {e: 0 for e in self.CE}
        self.dsem = {q: [nc.alloc_semaphore("d_%s%d" % (q, i)) for i in range(nslots)] for q in self.DQ}
        self.duse = {q: [0] * nslots for q in self.DQ}
        self.dnext = {q: 0 for q in self.DQ}
        self.nslots = nslots
        self.n_emitted = 0

    def add(self, eng, fn, reads=(), writes=(), dma=False):
        self.ops.append((eng, fn, tuple(reads), tuple(writes), dma))

    def flush(self):
        ops = self.ops
        self.ops = []
        if not ops:
            return
        n = len(ops)
        last_w = {}
        readers = {}
        deps = [None] * n
        signaled = [False] * n
        for i, (eng, fn, reads, writes, dma) in enumerate(ops):
            d = set()
            for k in reads:
                if k in last_w:
                    d.add(last_w[k])
            for k in writes:
                if k in last_w:
                    d.add(last_w[k])
                for r in readers.get(k, ()):
                    d.add(r)
            d.discard(i)
            dd = []
            for j in d:
                je, _, _, _, jd = ops[j]
                if (not jd) and je == "pe" and eng == "pe" and not dma:
                    continue
                dd.append(j)
                if not jd:
                    signaled[j] = True
            deps[i] = dd
            for k in writes:
                last_w[k] = i
                readers[k] = []
            for k in reads:
                readers.setdefault(k, []).append(i)
        token = [None] * n
        pre_wait = [None] * n
        ccount = dict(self.ccount)
        for i, (eng, fn, reads, writes, dma) in enumerate(ops):
            if dma:
                q = eng
                s = self.dnext[q]
                self.dnext[q] = (s + 1) % self.nslots
                prev = self.duse[q][s]
                self.duse[q][s] = prev + 1
                pre_wait[i] = (("d", q, s), 16 * prev)
                token[i] = (("d", q, s), 16 * (prev + 1))
            elif signaled[i]:
                ccount[eng] += 1
                token[i] = (("c", eng), ccount[eng])
        start_vals = dict(self.ccount)
        self.ccount = ccount
        base_known = {}
        for e in self.CE:
            base_known[("c", e)] = start_vals[e]
        prev_duse = {}
        per_eng = {e: [] for e in ("pe", "act", "dve", "pool", "sp")}
        for i, op in enumerate(ops):
            per_eng[op[0]].append(i)
        uses_this = {q: [0] * self.nslots for q in self.DQ}
        for i, op in enumerate(ops):
            if op[4]:
                (_, q, s), v = token[i]
                uses_this[q][s] += 1
        for q in self.DQ:
            for s in range(self.nslots):
                base_known[("d", q, s)] = 16 * (self.duse[q][s] - uses_this[q][s])

        def semh(key):
            return self.csem[key[1]] if key[0] == "c" else self.dsem[key[1]][key[2]]

        nc = self.nc
        first_flush = self.n_emitted == 0
        self.n_emitted += 1

        def emit_engine(eng_name, e):
            known = {}
            if not first_flush:
                for key, v in base_known.items():
                    if v > 0:
                        e.wait_ge(semh(key), v)
            known.update(base_known)
            for i in per_eng[eng_name]:
                _, fn, reads, writes, dma = ops[i]
                if pre_wait[i] is not None:
                    key, v = pre_wait[i]
                    if known.get(key, 0) < v:
                        e.wait_ge(semh(key), v)
                        known[key] = v
                for j in deps[i]:
                    key, v = token[j]
                    if known.get(key, 0) < v:
                        e.wait_ge(semh(key), v)
                        known[key] = v
                ins = fn(e)
                if token[i] is not None:
                    key, v = token[i]
                    ins.then_inc(semh(key), 16 if key[0] == "d" else 1)

        with nc.Block() as block:
            @block.tensor
            def _(e):
                emit_engine("pe", e)

            @block.scalar
            def _(e):
                emit_engine("act", e)

            @block.vector
            def _(e):
                emit_engine("dve", e)

            @block.gpsimd
            def _(e):
                emit_engine("pool", e)

            @block.sync
            def _(e):
                emit_engine("sp", e)

    def final_wait(self):
        nc = self.nc
        with nc.Block() as block:
            @block.sync
            def _(e):
                for en in self.CE:
                    if self.ccount[en] > 0:
                        e.wait_ge(self.csem[en], self.ccount[en])
                for q in self.DQ:
                    for s in range(self.nslots):
                        if self.duse[q][s] > 0:
                            e.wait_ge(self.dsem[q][s], 16 * self.duse[q][s])


class Arena:
    def __init__(self, nc, name, nbytes):
        self.t = nc.alloc_sbuf_tensor(name, [128, nbytes // 4], F32)
        self.cap = nbytes
        self.off = 0
        self.name = name
        self.n = 0

    def reset(self):
        self.off = 0

    def alloc(self, free_shape, dtype):
        sz = DT_SIZE[dtype]
        nel = int(np.prod(free_shape))
        nb = (nel * sz + 31) // 32 * 32
        assert self.off + nb <= self.cap, ("SBUF arena overflow", self.name, self.off, nb, self.cap)
        ap = self.t[:, self.off // 4:(self.off + nb) // 4]
        if dtype != F32:
            ap = ap.bitcast(dtype)
        ap = ap[:, 0:nel]
        if len(free_shape) == 2:
            ap = ap.rearrange("p (a b) -> p a b", a=free_shape[0])
        elif len(free_shape) == 3:
            ap = ap.rearrange("p (a b c) -> p a b c", a=free_shape[0], b=free_shape[1])
        self.off += nb
        self.n += 1
        return ap, "%s_%d_%d" % (self.name, self.off, self.n)


import ml_dtypes

EPS = 1e-6


class Cfg:
    def __init__(self, NB=2, S=2048, L=256, D=2048, DFF=5632, EFF=7168, E=8):
        self.NB, self.S, self.L, self.D, self.DFF, self.EFF, self.E = NB, S, L, D, DFF, EFF, E
        self.TOKB = S + L
        self.T = NB * self.TOKB
        self.KC = D // 128
        self.TB = min(512, S)


def vec_layout(cfg):
    KC = cfg.KC
    names = [("mixg0", KC), ("ffng0", KC), ("mixg1", KC), ("ffng1", KC),
             ("adab0", 6 * KC), ("adab1", 6 * KC),
             ("qag", 4), ("kvag", 2), ("qng_n", 1), ("qng_r", 1), ("kng_n", 1), ("kng_r", 1),
             ("dww", 8 * 31), ("dwb", 8), ("lng", 8), ("lnb", 8),
             ("sqg", 1), ("skg", 1), ("sink", 12), ("rw", KC * 8)]
    off = {}
    o = 0
    for n, w in names:
        off[n] = (o, w)
        o += w
    return off, o


class Builder:
    def __init__(self, cfg, layers=(0, 1)):
        self.cfg = cfg
        self.layers = layers
        nc = self.nc = bass.Bass("TRN2", target_bir_lowering=False)
        c = cfg
        self.P = Prog(nc)
        self.A = Arena(nc, "arena", 190 * 1024)
        self.Pers = Arena(nc, "pers", 12 * 1024)
        self.ps = [nc.alloc_psum_tensor("ps%d" % i, [128, 512], F32).ap() for i in range(8)]
        self.psn = 0
        self.voff, self.nv = vec_layout(cfg)
        D, KC = c.D, c.KC

        def ext(name, shape, dt=F32):
            return nc.dram_tensor(name, list(shape), dt, kind="ExternalInput").ap()

        def scr(name, shape, dt):
            return nc.dram_tensor(name, list(shape), dt).ap()

        self.xT_in = ext("xT", [c.NB, D, c.S])
        self.cT_in = ext("cT", [c.NB, D, c.L])
        self.cvec = ext("cvec", [D, c.NB + 1])
        self.vecs_in = ext("vecs", [128, self.nv])
        self.mats_in = ext("mats", [128, 6, 128], BF16)
        self.matsf_in = ext("matsf", [128, 4, 128])
        self.rope_in = ext("rope", [128, 4, c.TOKB])
        self.wmask_in = ext("wmask", [128, 2, 128], BF16)
        self.dftc_in = ext("dftc", [128, 256], BF16)
        self.dftT_in = ext("dftT", [2, c.S, c.S], BF16)
        self.selm_in = ext("selm", [8, 8, 128])
        self.outT = nc.dram_tensor("outT", [c.NB, D, c.S], F32, kind="ExternalOutput").ap()
        W = {}
        Wshapes = {"ada0": (D, 6 * D), "ada1": (D, 6 * D), "win0": (D, 2944), "wqb": (512, 1536), "wkvb": (256, 2048),
                   "wout0": (2048, D), "dg": (D, c.DFF), "du": (D, c.DFF), "dd": (c.DFF, D),
                   "win1": (D, 3072), "wout1": (2048, D)}
        for e in range(c.E):
            Wshapes["eg%d" % e] = (D, c.EFF)
            Wshapes["eu%d" % e] = (D, c.EFF)
            Wshapes["ed%d" % e] = (c.EFF, D)
        self.Wshapes = Wshapes
        self.Wext = {}
        self.Wbf = {}
        for k, shp in Wshapes.items():
            self.Wext[k] = ext("w_" + k, shp)
            self.Wbf[k] = scr("b_" + k, shp, BF16)
        T = c.T
        self.xs = scr("xs", [D, T], F32)
        self.hT = scr("hT", [1024, T], F32)
        self.qT = scr("qT", [1536, T], BF16)
        self.kT = scr("kT", [1536, T], BF16)
        self.Vt = scr("Vt", [T, 1024], BF16)
        self.mixT = scr("mixT", [2048, T], BF16)
        self.fT = scr("fT", [512, T], BF16)

    def psum(self):
        lo = getattr(self, "ps_lo", 0)
        if self.psn < lo:
            self.psn = lo
        i = self.psn
        self.psn = self.psn + 1
        if self.psn >= 8:
            self.psn = lo
        return self.ps[i], "ps%d" % i

    def dma(self, out, in_, reads, writes, q="sp"):
        self.P.add(q, lambda e: e.dma_start(out=out, in_=in_), reads=reads, writes=writes, dma=True)

    def vec(self, name, j=0, n=1):
        o, w = self.voff[name]
        return self.vecs[:, o + j:o + j + n]

    def stage_prep(self):
        c = self.cfg
        P = self.P
        self.deferred = []
        for k, shp in self.Wshapes.items():
            if k in ("ada1", "win1", "wout1") or k[0] == "e":
                if 1 not in self.layers:
                    continue
            if k[0] == "e" and k not in ("even",):
                self.deferred.append(k)
                continue
            self.cast_weight(k)
        self.vecs, kv = self.Pers.alloc((self.nv,), F32)
        self.mats, km = self.Pers.alloc((6, 128), BF16)
        self.matsf, kf = self.Pers.alloc((4, 128), F32)
        self.dma(self.vecs, self.vecs_in, [], ["vecs"])
        self.dma(self.mats, self.mats_in, [], ["mats"])
        self.dma(self.matsf, self.matsf_in, [], ["matsf"])
        self.mod, _ = self.Pers.alloc((6 * c.KC, c.NB + 1), F32)
        self.moda, _ = self.Pers.alloc((2 * c.KC, c.NB + 1), F32)
        self.gsc, _ = self.Pers.alloc((16,), F32)
        P.flush()

    def cast_weight(self, k):
        shp = self.Wshapes[k]
        rows = shp[0]
        step = max(128, (8 << 20) // (shp[1] * 4) // 128 * 128)
        for r0 in range(0, rows, step):
            r1 = min(rows, r0 + step)
            self.dma(self.Wbf[k][r0:r1, :], self.Wext[k][r0:r1, :], reads=[], writes=["W" + k], q="pool")

    def cast_some(self, n):
        for _ in range(n):
            if self.deferred:
                self.cast_weight(self.deferred.pop(0))

    def gemm_fm(self, wname, K, c0, c1, rhs, rhs_keys, N, evac, arena=None, krow0=0, evac_group=None):
        P = self.P
        Wd = self.Wbf[wname]
        KCn = K // 128
        if not hasattr(self, "slabs") or self.slabs is None:
            A = arena or self.A
            self.slabs = [A.alloc((16, 512), BF16) for _ in range(2)]
            self.slabn = 0
        for g0 in range(c0, c1, 512):
            gc = min(512, c1 - g0)
            nch = (gc + 127) // 128
            banks = [self.psum() for _ in range(nch)]
            for s0 in range(0, KCn, 16):
                skc = min(16, KCn - s0)
                slab, sk = self.slabs[self.slabn]
                self.slabn ^= 1
                src = Wd[krow0 + s0 * 128:krow0 + (s0 + skc) * 128, g0:g0 + gc].rearrange("(kc p) n -> p kc n", p=128)
                self.dma(slab[:, 0:skc, 0:gc], src, ["W" + wname], [sk])
                for ci in range(nch):
                    rows = min(128, gc - ci * 128)
                    pb, pk = banks[ci]

                    def mm(e, ci=ci, rows=rows, pb=pb, slab=slab, s0=s0, skc=skc):
                        m = None
                        for kc in range(skc):
                            m = e.matmul(pb[0:rows, 0:N], lhsT=slab[:, kc, ci * 128:ci * 128 + rows], rhs=rhs(s0 + kc),
                                         start=(s0 + kc == 0), stop=(s0 + kc == KCn - 1))
                        return m
                    P.add("pe", mm, reads=[sk] + list(rhs_keys), writes=[pk])
            if evac_group is not None:
                evac_group([((g0 + ci * 128) // 128, banks[ci][0][0:min(128, gc - ci * 128), 0:N], banks[ci][1]) for ci in range(nch)])
                continue
            for ci in range(nch):
                rows = min(128, gc - ci * 128)
                pb, pk = banks[ci]
                evac((g0 + ci * 128) // 128, pb[0:rows, 0:N], pk, rows)

    def gemm_tm(self, wname, K, c0, c1, act, act_keys, ntok, evac):
        P = self.P
        Wd = self.Wbf[wname]
        KCn = K // 128
        assert KCn <= 16
        for g0 in range(c0, c1, 512):
            gc = min(512, c1 - g0)
            slab, sk = self.slabs[self.slabn]
            self.slabn ^= 1
            src = Wd[:, g0:g0 + gc].rearrange("(kc p) n -> p kc n", p=128)
            self.dma(slab[:, 0:KCn, 0:gc], src, ["W" + wname], [sk])
            for t0 in range(0, ntok, 128):
                tn = min(128, ntok - t0)
                pb, pk = self.psum()

                def mm(e, pb=pb, slab=slab, t0=t0, tn=tn, gc=gc):
                    m = None
                    for kc in range(KCn):
                        m = e.matmul(pb[0:tn, 0:gc], lhsT=act[:, kc, t0:t0 + tn], rhs=slab[:, kc, 0:gc], start=(kc == 0), stop=(kc == KCn - 1))
                    return m
                P.add("pe", mm, reads=[sk] + list(act_keys), writes=[pk])
                evac(g0, t0, tn, pb[0:tn, 0:gc], pk, gc)

    def ssq(self, terms, N, keys):
        pb, pk = self.psum()

        def mm(e):
            m = None
            for i, (mi, ap) in enumerate(terms):
                m = e.matmul(pb[:, 0:N], lhsT=self.mats[:, mi, :], rhs=ap, start=(i == 0), stop=(i == len(terms) - 1))
            return m
        self.P.add("pe", mm, reads=list(keys) + ["mats"], writes=[pk])
        return pb, pk

    def rstd_from(self, pb, pk, N, epsn, out, ok):
        self.P.add("dve", lambda e: e.tensor_scalar(out=out, in0=pb[:, 0:N], scalar1=float(epsn), scalar2=None, op0=ALU.add), reads=[pk], writes=[ok])
        self.P.add("act", lambda e: e.activation(out=out, in_=out, func=AF.Ln), reads=[ok], writes=[ok])
        self.P.add("act", lambda e: e.activation(out=out, in_=out, func=AF.Exp, scale=-0.5), reads=[ok], writes=[ok])

    def stage_mod(self, layer):
        c = self.cfg
        P = self.P
        A = self.A
        A.reset()
        self.slabs = None
        KC, NBp = c.KC, c.NB + 1
        cb, kcb = A.alloc((KC, NBp), F32)
        ch, kch = A.alloc((KC, NBp), BF16)
        self.dma(cb, self.cvec.rearrange("(kc p) n -> p kc n", p=128), [], [kcb])
        P.add("act", lambda e: e.activation(out=ch, in_=cb, func=AF.Silu), reads=[kcb], writes=[kch])
        bname = "adab%d" % layer
        mod = self.mod

        def evac(ch_i, ps_ap, pk, rows):
            P.add("act", lambda e: e.activation(out=mod[:, ch_i, :], in_=ps_ap, func=AF.Identity, bias=self.vec(bname, ch_i), scale=1.0),
                  reads=[pk, "vecs"], writes=["mod"])
        self.gemm_fm("ada%d" % layer, c.D, 0, 6 * c.D, lambda kc: ch[:, kc, :], [kch], NBp, evac)
        sq = float(np.sqrt(c.D))
        for which, (comp, gname) in enumerate(((1, "mixg%d" % layer), (4, "ffng%d" % layer))):
            dst = self.moda[:, which * KC:(which + 1) * KC, :]
            src = mod[:, comp * KC:(comp + 1) * KC, :]
            P.add("dve", lambda e, dst=dst, src=src: e.tensor_scalar(out=dst, in0=src, scalar1=1.0, scalar2=sq, op0=ALU.add, op1=ALU.mult),
                  reads=["mod"], writes=["moda%d" % which])
            for j in range(NBp):
                P.add("dve", lambda e, dst=dst, j=j, gname=gname: e.tensor_tensor(out=dst[:, :, j], in0=dst[:, :, j], in1=self.vec(gname, 0, KC), op=ALU.mult),
                      reads=["moda%d" % which, "vecs"], writes=["moda%d" % which])
        P.flush()

    def norm_mod(self, xb, kx, N, which, mi, u, ku, sqb, ksq, rst, krst, tmps, router=None):
        c = self.cfg
        P = self.P
        KC = c.KC
        P.add("act", lambda e: e.activation(out=sqb[:, :, 0:N], in_=xb[:, :, 0:N], func=AF.Square), reads=[kx], writes=[ksq])
        pb, pk = self.ssq([(0, sqb[:, kc, 0:N]) for kc in range(KC)], N, [ksq])
        self.rstd_from(pb, pk, N, c.D * EPS, rst[:, 0:N], krst)
        shc = 0 if which == 0 else 3
        for kc in range(KC):
            tp, tk = tmps[kc % len(tmps)]
            P.add("dve", lambda e, kc=kc, tp=tp: e.tensor_tensor(out=tp[:, 0:N], in0=xb[:, kc, 0:N], in1=rst[:, 0:N], op=ALU.mult),
                  reads=[kx, krst], writes=[tk])
            if router is None:
                P.add("act", lambda e, kc=kc, tp=tp: e.activation(out=u[:, kc, 0:N], in_=tp[:, 0:N], func=AF.Identity,
                                                            scale=self.moda[:, which * KC + kc, mi:mi + 1], bias=self.mod[:, shc * KC + kc, mi:mi + 1]),
                      reads=[tk, "moda%d" % which, "mod"], writes=[ku + "_%d" % kc])
            else:
                pr, kpr = router
                P.add("act", lambda e, kc=kc, tp=tp: e.activation(out=tp[:, 0:N], in_=tp[:, 0:N], func=AF.Identity,
                                                            scale=self.moda[:, which * KC + kc, mi:mi + 1], bias=self.mod[:, shc * KC + kc, mi:mi + 1]),
                      reads=[tk, "moda%d" % which, "mod"], writes=[tk])
                P.add("pool", lambda e, kc=kc, tp=tp: e.tensor_copy(out=u[:, kc, 0:N], in_=tp[:, 0:N]), reads=[tk], writes=[ku + "_%d" % kc])
                rwv = self.vec("rw", kc * 8, 8)
                P.add("pe", lambda e, kc=kc, tp=tp, rwv=rwv: e.matmul(pr[0:8, 0:N], lhsT=rwv, rhs=tp[:, 0:N], start=(kc == 0), stop=(kc == KC - 1)),
                      reads=[tk, "vecs"], writes=[kpr])

    def blocks(self, tb=None):
        c = self.cfg
        tb = tb or c.TB
        out = []
        for b in range(c.NB):
            for t0 in range(0, c.S, tb):
                out.append((b, b * c.TOKB + t0, min(tb, c.S - t0), False, t0))
            out.append((b, b * c.TOKB + c.S, c.L, True, c.S))
        return out

    def load_x(self, layer, b, col0, N, is_ctx, pos0, xb, kx):
        c = self.cfg
        if layer == 0:
            src = self.cT_in[b] if is_ctx else self.xT_in[b][:, pos0:pos0 + N]
            if is_ctx:
                src = src[:, 0:N]
            rk = []
        else:
            src = self.xs[:, col0:col0 + N]
            rk = ["xs"]
        self.dma(xb[:, :, 0:N], src.rearrange("(kc p) n -> p kc n", p=128), rk, [kx])

    def rope_apply(self, src, ksrc, N, pidx, cosT, sinT, pos0, out, kout, tmp, ktmp):
        P = self.P
        pb, pk = self.psum()
        P.add("pe", lambda e: e.matmul(pb[:, 0:N], lhsT=self.matsf[:, pidx, :], rhs=src, start=True, stop=True), reads=[ksrc, "matsf"], writes=[pk])
        P.add("dve", lambda e: e.tensor_tensor(out=tmp[:, 0:N], in0=pb[:, 0:N], in1=sinT[:, pos0:pos0 + N], op=ALU.mult), reads=[pk, "rope"], writes=[ktmp])
        P.add("pool", lambda e: e.tensor_tensor(out=src, in0=src, in1=cosT[:, pos0:pos0 + N], op=ALU.mult), reads=[ksrc, "rope", pk], writes=[ksrc])
        P.add("dve", lambda e: e.tensor_tensor(out=out, in0=src, in1=tmp[:, 0:N], op=ALU.add), reads=[ksrc, ktmp], writes=[kout])

    def stage_L0A(self):
        c = self.cfg
        P, A = self.P, self.A
        A.reset()
        self.slabs = None
        self.cast_some(5)
        KC, TB = c.KC, min(256, c.TB)
        xb, kx = A.alloc((KC, TB), F32)
        sqb, ksq = A.alloc((KC, TB), BF16)
        u, ku = A.alloc((KC, TB), BF16)
        rst, krst = A.alloc((TB,), F32)
        tmps = [A.alloc((TB,), F32) for _ in range(3)]
        qa, kqa = A.alloc((6, TB), F32)
        qasq, kqasq = A.alloc((6, TB), BF16)
        qn, kqn = A.alloc((6, TB), BF16)
        cva, kcva = A.alloc((8, TB), F32)
        sg, ksg = A.alloc((TB,), F32)
        hb, khb = A.alloc((2, TB), F32)
        kpe, kkpe = A.alloc((TB,), F32)
        qr, kqr = A.alloc((12, TB), F32)
        qsq, kqsq = A.alloc((12, TB), BF16)
        qo, kqo = A.alloc((12, TB), BF16)
        vb, kvb = A.alloc((max(2, (max(TB, c.L) + 127) // 128), 1024), BF16)
        self.hts = [A.alloc((TB,), F32) for _ in range(12)]
        rope, _ = A.alloc((2, c.TOKB), F32)
        self.dma(rope, self.rope_in[:, 0:2, :], [], ["rope"])
        cosT, sinT = rope[:, 0, :], rope[:, 1, :]
        g = self.gsc
        sc = float(192 ** -0.5)
        for (dst, src, n, f) in ((0, "qag", 4, 512 ** 0.5), (4, "kvag", 2, 256 ** 0.5), (6, "qng_n", 1, 192 ** 0.5 * sc), (7, "qng_r", 1, 192 ** 0.5 * sc),
                                 (8, "kng_n", 1, 192 ** 0.5), (9, "kng_r", 1, 192 ** 0.5)):
            P.add("dve", lambda e, dst=dst, src=src, n=n, f=f: e.tensor_scalar(out=g[:, dst:dst + n], in0=self.vec(src, 0, n), scalar1=float(f), scalar2=None, op0=ALU.mult),
                  reads=["vecs"], writes=["gsc"])
        for (b, col0, N, is_ctx, pos0) in self.blocks(TB):
            mi = c.NB if is_ctx else b
            self.load_x(0, b, col0, N, is_ctx, pos0, xb, kx)
            self.norm_mod(xb, kx, N, 0, mi, u, ku, sqb, ksq, rst, krst, tmps)
            ukeys = [ku + "_%d" % kc for kc in range(KC)]

            def evac(ch, ps_ap, pk, rows, N=N, col0=col0):
                if ch < 6:
                    P.add("act", lambda e: e.activation(out=qa[:, ch, 0:N], in_=ps_ap, func=AF.Copy), reads=[pk], writes=[kqa + str(ch)])
                elif ch < 14:
                    P.add("act", lambda e: e.activation(out=cva[:, ch - 6, 0:N], in_=ps_ap, func=AF.Copy), reads=[pk], writes=[kcva + str(ch)])
                elif ch < 22:
                    j = ch - 14
                    hbj = hb[:, j % 2, 0:N]
                    P.add("act", lambda e: e.activation(out=sg[:, 0:N], in_=ps_ap, func=AF.Sigmoid), reads=[pk], writes=[ksg])
                    P.add("dve", lambda e: e.tensor_tensor(out=hbj, in0=sg[:, 0:N], in1=cva[:, j, 0:N], op=ALU.mult), reads=[ksg, kcva + str(j + 6)], writes=[khb + str(j % 2)])
                    self.dma(self.hT[j * 128:(j + 1) * 128, col0:col0 + N], hbj, [khb + str(j % 2)], ["hT"])
                else:
                    P.add("act", lambda e: e.activation(out=kpe[:, 0:N], in_=ps_ap, func=AF.Copy), reads=[pk], writes=[kkpe])
            self.gemm_fm("win0", c.D, 0, 2944, lambda kc, N=N: u[:, kc, 0:N], ukeys, N, evac)
            P.add("act", lambda e, N=N: e.activation(out=qasq[:, :, 0:N], in_=qa[:, :, 0:N], func=AF.Square), reads=[kqa + str(i) for i in range(6)], writes=[kqasq])
            for (lo, hi, epsn) in ((0, 4, 512 * EPS), (4, 6, 256 * EPS)):
                pb, pk = self.ssq([(0, qasq[:, kc, 0:N]) for kc in range(lo, hi)], N, [kqasq])
                self.rstd_from(pb, pk, N, epsn, rst[:, 0:N], krst)
                for kc in range(lo, hi):
                    P.add("dve", lambda e, kc=kc, N=N: e.scalar_tensor_tensor(out=qn[:, kc, 0:N], in0=qa[:, kc, 0:N], scalar=g[:, kc:kc + 1], in1=rst[:, 0:N], op0=ALU.mult, op1=ALU.mult),
                          reads=[kqa + str(kc), krst, "gsc"], writes=[kqn + str(kc)])
            def evq(ch, ps_ap, pk, rows, N=N):
                P.add("act", lambda e: e.activation(out=qr[:, ch, 0:N], in_=ps_ap, func=AF.Copy), reads=[pk], writes=[kqr + str(ch)])
            self.gemm_fm("wqb", 512, 0, 1536, lambda kc, N=N: qn[:, kc, 0:N], [kqn + str(i) for i in range(4)], N, evq)
            self.head_norm_rope(qr, kqr, qsq, kqsq, qo, kqo, N, 6, 7, cosT, sinT, pos0, rst, krst, tmps, None, None)
            self.dma(self.qT[:, col0:col0 + N].rearrange("(ch p) n -> p ch n", p=128), qo[:, :, 0:N], [kqo + str(i) for i in range(12)], ["qT"])
            def evk(ch, ps_ap, pk, rows, N=N):
                P.add("act", lambda e: e.activation(out=qr[:, ch, 0:N], in_=ps_ap, func=AF.Copy), reads=[pk], writes=[kqr + str(ch)])
            self.gemm_fm("wkvb", 256, 0, 1024, lambda kc, N=N: qn[:, 4 + kc, 0:N], [kqn + "4", kqn + "5"], N, evk)
            self.head_norm_rope(qr, kqr, qsq, kqsq, qo, kqo, N, 8, 9, cosT, sinT, pos0, rst, krst, tmps, kpe, kkpe)
            self.dma(self.kT[:, col0:col0 + N].rearrange("(ch p) n -> p ch n", p=128), qo[:, :, 0:N], [kqo + str(i) for i in range(12)], ["kT"])

            def evv(g0, t0, tn, ps_ap, pk, gc, col0=col0):
                ti = t0 // 128
                P.add("act", lambda e: e.activation(out=vb[0:tn, ti, g0 - 1024:g0 - 1024 + gc], in_=ps_ap, func=AF.Copy), reads=[pk], writes=[kvb + "%d_%d" % (ti, g0)])
                if g0 + gc >= 2048:
                    self.dma(self.Vt[col0 + t0:col0 + t0 + tn, :], vb[0:tn, ti, :], [kvb + "%d_%d" % (ti, gg) for gg in (1024, 1536)], ["Vt"])
            self.gemm_tm("wkvb", 256, 1024, 2048, qn[:, 4:6, :], [kqn + "4", kqn + "5"], N, evv)
        P.flush()

    def head_norm_rope(self, qr, kqr, qsq, kqsq, qo, kqo, N, gn, gr, cosT, sinT, pos0, rst, krst, tmps, kpe, kkpe):
        P = self.P
        g = self.gsc
        is_k = kpe is not None
        nsq = 8 if is_k else 12
        P.add("act", lambda e: e.activation(out=qsq[:, 0:nsq, 0:N], in_=qr[:, 0:nsq, 0:N], func=AF.Square), reads=[kqr + str(i) for i in range(nsq)], writes=[kqsq])
        if is_k:
            P.add("act", lambda e: e.activation(out=qsq[:, 8, 0:N], in_=kpe[:, 0:N], func=AF.Square), reads=[kkpe], writes=[kqsq + "p"])
            P.add("dve", lambda e: e.tensor_scalar(out=qr[:, 8, 0:N], in0=kpe[:, 0:N], scalar1=g[:, gr:gr + 1], scalar2=None, op0=ALU.mult), reads=[kkpe, "gsc"], writes=[kqr + "8"])
            tp, tk = tmps[0]
            pb, pk = self.psum()
            src = qr[:, 8, 0:N]
            P.add("pe", lambda e: e.matmul(pb[:, 0:N], lhsT=self.matsf[:, 0, :], rhs=src, start=True, stop=True), reads=[kqr + "8", "matsf"], writes=[pk])
            P.add("dve", lambda e: e.tensor_tensor(out=tp[:, 0:N], in0=pb[:, 0:N], in1=sinT[:, pos0:pos0 + N], op=ALU.mult), reads=[pk, "rope"], writes=[tk])
            P.add("dve", lambda e: e.tensor_tensor(out=src, in0=src, in1=cosT[:, pos0:pos0 + N], op=ALU.mult), reads=[kqr + "8", "rope", pk], writes=[kqr + "8"])
            P.add("dve", lambda e: e.tensor_tensor(out=src, in0=src, in1=tp[:, 0:N], op=ALU.add), reads=[kqr + "8", tk], writes=[kqr + "8"])
        hts = self.hts
        hp_ = []
        for h in range(8):
            rsq = qsq[:, 8, 0:N] if is_k else qsq[:, 8 + h // 2, 0:N]
            rkey = (kqsq + "p") if is_k else kqsq
            topbot = 1 if (is_k or h % 2 == 0) else 2
            hp_.append(self.ssq([(0, qsq[:, h, 0:N]), (topbot, rsq)], N, [kqsq, rkey]))
        for h in range(8):
            tp, tk = hts[h]
            P.add("dve", lambda e, h=h, tp=tp: e.tensor_scalar(out=tp[:, 0:N], in0=hp_[h][0][:, 0:N], scalar1=float(192 * EPS), scalar2=None, op0=ALU.add), reads=[hp_[h][1]], writes=[tk])
        pp_ = []
        for j in range(4):
            rsq = qsq[:, 8, 0:N] if is_k else qsq[:, 8 + j, 0:N]
            pp_.append(self.ssq([(3, qsq[:, 2 * j, 0:N]), (4, qsq[:, 2 * j + 1, 0:N]), (5, rsq)], N, [kqsq, kqsq + "p"] if is_k else [kqsq]))
        for j in range(4):
            tp, tk = hts[8 + j]
            P.add("dve", lambda e, j=j, tp=tp: e.tensor_scalar(out=tp[:, 0:N], in0=pp_[j][0][:, 0:N], scalar1=float(192 * EPS), scalar2=None, op0=ALU.add), reads=[pp_[j][1]], writes=[tk])
        for i in range(12):
            tp, tk = hts[i]
            P.add("act", lambda e, tp=tp: e.activation(out=tp[:, 0:N], in_=tp[:, 0:N], func=AF.Ln), reads=[tk], writes=[tk])
        for i in range(12):
            tp, tk = hts[i]
            P.add("act", lambda e, tp=tp: e.activation(out=tp[:, 0:N], in_=tp[:, 0:N], func=AF.Exp, scale=-0.5), reads=[tk], writes=[tk])
        for h in range(8):
            tp, tk = hts[h]
            P.add("dve", lambda e, h=h, tp=tp: e.scalar_tensor_tensor(out=qo[:, h, 0:N], in0=qr[:, h, 0:N], scalar=g[:, gn:gn + 1], in1=tp[:, 0:N], op0=ALU.mult, op1=ALU.mult),
                  reads=[kqr + str(h), tk, "gsc"], writes=[kqo + str(h)])
        if is_k:
            for j in range(4):
                tp, tk = hts[8 + j]
                P.add("dve", lambda e, j=j, tp=tp: e.tensor_tensor(out=qo[:, 8 + j, 0:N], in0=qr[:, 8, 0:N], in1=tp[:, 0:N], op=ALU.mult),
                      reads=[kqr + "8", tk], writes=[kqo + str(8 + j)])
        else:
            for j in range(4):
                tp, tk = hts[8 + j]
                src = qr[:, 8 + j, 0:N]
                P.add("dve", lambda e, src=src, tp=tp: e.scalar_tensor_tensor(out=src, in0=src, scalar=g[:, gr:gr + 1], in1=tp[:, 0:N], op0=ALU.mult, op1=ALU.mult),
                      reads=[kqr + str(8 + j), tk, "gsc"], writes=[kqr + str(8 + j)])
            pr_ = []
            for j in range(4):
                src = qr[:, 8 + j, 0:N]
                pb2, pk2 = self.psum()
                pr_.append((pb2, pk2))
                P.add("pe", lambda e, src=src, pb2=pb2: e.matmul(pb2[:, 0:N], lhsT=self.matsf[:, 0, :], rhs=src, start=True, stop=True), reads=[kqr + str(8 + j), "matsf"], writes=[pk2])
            for j in range(4):
                tp, tk = hts[8 + j]
                src = qr[:, 8 + j, 0:N]
                pb2, pk2 = pr_[j]
                P.add("dve", lambda e, pb2=pb2, tp=tp: e.tensor_tensor(out=tp[:, 0:N], in0=pb2[:, 0:N], in1=sinT[:, pos0:pos0 + N], op=ALU.mult), reads=[pk2, "rope", tk], writes=[tk])
                P.add("dve", lambda e, src=src: e.tensor_tensor(out=src, in0=src, in1=cosT[:, pos0:pos0 + N], op=ALU.mult), reads=[kqr + str(8 + j), "rope", pk2], writes=[kqr + str(8 + j)])
            for j in range(4):
                tp, tk = hts[8 + j]
                src = qr[:, 8 + j, 0:N]
                P.add("dve", lambda e, src=src, tp=tp, j=j: e.tensor_tensor(out=qo[:, 8 + j, 0:N], in0=src, in1=tp[:, 0:N], op=ALU.add), reads=[kqr + str(8 + j), tk], writes=[kqo + str(8 + j)])

    def stage_L0B(self, merged=False):
        c = self.cfg
        P, A = self.P, self.A
        if not merged:
            A.reset()
            self.slabs = None
        self.cast_some(4)
        NKC = c.TOKB // 128
        TB = c.TB
        kn, kkn = A.alloc((c.TOKB,), BF16)
        kr, kkr = A.alloc((c.TOKB,), BF16)
        vh, kvh = A.alloc((NKC, 128), BF16)
        qn, kqn = A.alloc((TB,), BF16)
        qr, kqr = A.alloc((TB,), BF16)
        pts = [A.alloc((TB,), BF16) for _ in range(4)]
        rden, krden = A.alloc((TB,), F32)
        ob, kob = A.alloc((TB,), BF16)
        self.ps_lo = 2
        self.psn = 2
        pO, kO, pD, kD = self.ps[0], "ps0", self.ps[1], "ps1"
        for b in range(c.NB):
            cb = b * c.TOKB
            for h in range(8):
                yield
                hp = (h % 2) * 64
                self.dma(kn, self.kT[h * 128:(h + 1) * 128, cb:cb + c.TOKB], ["kT"], [kkn])
                self.dma(kr, self.kT[1024 + (h // 2) * 128:1024 + (h // 2) * 128 + 128, cb:cb + c.TOKB], ["kT"], [kkr])
                self.dma(vh, self.Vt[cb:cb + c.TOKB, h * 128:(h + 1) * 128].rearrange("(kc p) d -> p kc d", p=128), ["Vt"], [kvh])
                for (bb, col0, N, is_ctx, pos0) in self.blocks():
                    if bb != b:
                        continue
                    self.dma(qn[:, 0:N], self.qT[h * 128:(h + 1) * 128, col0:col0 + N], ["qT"], [kqn])
                    self.dma(qr[:, 0:N], self.qT[1024 + (h // 2) * 128:1024 + (h // 2) * 128 + 128, col0:col0 + N], ["qT"], [kqr])
                    kcs = list(range(c.S // 128, NKC)) if is_ctx else list(range(NKC))
                    pend = []
                    for i, kc in enumerate(kcs):
                        pS, kS = self.psum()
                        pt, kpt = pts[i % 4]

                        def mmS(e, pS=pS, kc=kc, N=N):
                            e.matmul(pS[:, 0:N], lhsT=kn[:, kc * 128:(kc + 1) * 128], rhs=qn[:, 0:N], start=True, stop=False)
                            return e.matmul(pS[:, 0:N], lhsT=kr[hp:hp + 64, kc * 128:(kc + 1) * 128], rhs=qr[hp:hp + 64, 0:N], start=False, stop=True)
                        P.add("pe", mmS, reads=[kkn, kkr, kqn, kqr], writes=[kS])
                        P.add("act", lambda e, pS=pS, pt=pt, N=N: e.activation(out=pt[:, 0:N], in_=pS[:, 0:N], func=AF.Exp), reads=[kS], writes=[kpt])

                        def mmO(e, pt=pt, kc=kc, N=N, first=(i == 0), last=(i == len(kcs) - 1)):
                            e.matmul(pO[:, 0:N], lhsT=vh[:, kc, :], rhs=pt[:, 0:N], start=first, stop=last)
                            return e.matmul(pD[:, 0:N], lhsT=self.mats[:, 0, :], rhs=pt[:, 0:N], start=first, stop=last)
                        pend.append((mmO, kpt))
                        if len(pend) > 2:
                            fn, kp = pend.pop(0)
                            P.add("pe", fn, reads=[kvh, kp, "mats"], writes=[kO, kD])
                    for fn, kp in pend:
                        P.add("pe", fn, reads=[kvh, kp, "mats"], writes=[kO, kD])
                    P.add("dve", lambda e, N=N: e.reciprocal(out=rden[:, 0:N], in_=pD[:, 0:N]), reads=[kD], writes=[krden])
                    P.add("dve", lambda e, N=N: e.tensor_tensor(out=ob[:, 0:N], in0=pO[:, 0:N], in1=rden[:, 0:N], op=ALU.mult), reads=[kO, krden], writes=[kob])
                    self.dma(self.mixT[h * 128:(h + 1) * 128, col0:col0 + N], ob[:, 0:N], [kob], ["mixT"])
        if not merged:
            self.ps_lo = 0
            P.flush()
        yield

    def stage_L0C(self, merged=False):
        c = self.cfg
        P, A = self.P, self.A
        if not merged:
            A.reset()
            self.slabs = None
        S = c.S
        hp, khp = A.alloc((S + 30,), F32)
        acc2, kacc2 = A.alloc((S,), F32)
        cvb, kcvb = A.alloc((8, S), F32)
        sqt, ksqt = A.alloc((8, 512), F32)
        mean, kmean = A.alloc((512,), F32)
        rst, krst = A.alloc((512,), F32)
        t1, kt1 = A.alloc((512,), F32)
        yo, kyo = A.alloc((8, 512), BF16)
        for b in range(c.NB):
            for (pos0, ln) in ((0, S), (S, c.L)):
                col = b * c.TOKB + pos0
                for j in range(8):
                    yield
                    P.add("pool", lambda e: e.memset(hp[:, 0:15], 0.0), reads=[], writes=[khp])
                    P.add("pool", lambda e, ln=ln: e.memset(hp[:, 15 + ln:30 + ln], 0.0), reads=[], writes=[khp])
                    self.dma(hp[:, 15:15 + ln], self.hT[j * 128:(j + 1) * 128, col:col + ln], ["hT"], [khp])
                    acc = cvb[:, j, 0:ln]
                    w = lambda t, j=j: self.vec("dww", j * 31 + t)
                    P.add("dve", lambda e, acc=acc, ln=ln, j=j, w=w: e.tensor_scalar(out=acc, in0=hp[:, 0:ln], scalar1=w(0), scalar2=self.vec("dwb", j), op0=ALU.mult, op1=ALU.add),
                          reads=[khp, "vecs"], writes=[kcvb + str(j)])
                    for t in range(1, 31):
                        P.add("dve", lambda e, acc=acc, ln=ln, t=t, w=w: e.scalar_tensor_tensor(out=acc, in0=hp[:, t:t + ln], scalar=w(t), in1=acc, op0=ALU.mult, op1=ALU.add),
                              reads=[khp, "vecs", kcvb + str(j)], writes=[kcvb + str(j)])
                for t0 in range(0, ln, 512):
                    yield
                    N = min(512, ln - t0)
                    ck = [kcvb + str(j) for j in range(8)]
                    P.add("act", lambda e, t0=t0, N=N: e.activation(out=sqt[:, :, 0:N], in_=cvb[:, :, t0:t0 + N], func=AF.Square), reads=ck, writes=[ksqt + str(j) for j in range(8)])
                    p1, k1 = self.psum()
                    p2, k2 = self.psum()

                    def mm1(e, p1=p1, t0=t0, N=N):
                        m = None
                        for j in range(8):
                            m = e.matmul(p1[:, 0:N], lhsT=self.matsf[:, 3, :], rhs=cvb[:, j, t0:t0 + N], start=(j == 0), stop=(j == 7))
                        return m

                    def mm2(e, p2=p2, N=N):
                        m = None
                        for j in range(8):
                            m = e.matmul(p2[:, 0:N], lhsT=self.matsf[:, 3, :], rhs=sqt[:, j, 0:N], start=(j == 0), stop=(j == 7))
                        return m
                    P.add("pe", mm1, reads=ck + ["matsf"], writes=[k1])
                    P.add("pe", mm2, reads=[ksqt + str(j) for j in range(8)] + ["matsf"], writes=[k2])
                    P.add("dve", lambda e, p1=p1, N=N: e.tensor_scalar(out=mean[:, 0:N], in0=p1[:, 0:N], scalar1=1.0 / 1024, scalar2=None, op0=ALU.mult), reads=[k1], writes=[kmean])
                    P.add("dve", lambda e, N=N: e.tensor_tensor(out=t1[:, 0:N], in0=mean[:, 0:N], in1=mean[:, 0:N], op=ALU.mult), reads=[kmean], writes=[kt1])
                    P.add("dve", lambda e, p2=p2, N=N: e.scalar_tensor_tensor(out=rst[:, 0:N], in0=p2[:, 0:N], scalar=1.0 / 1024, in1=t1[:, 0:N], op0=ALU.mult, op1=ALU.subtract),
                          reads=[k2, kt1], writes=[krst])
                    P.add("dve", lambda e, N=N: e.tensor_scalar(out=rst[:, 0:N], in0=rst[:, 0:N], scalar1=EPS, scalar2=None, op0=ALU.add), reads=[krst], writes=[krst])
                    P.add("act", lambda e, N=N: e.activation(out=rst[:, 0:N], in_=rst[:, 0:N], func=AF.Ln), reads=[krst], writes=[krst])
                    P.add("act", lambda e, N=N: e.activation(out=rst[:, 0:N], in_=rst[:, 0:N], func=AF.Exp, scale=-0.5), reads=[krst], writes=[krst])
                    for j in range(8):
                        P.add("dve", lambda e, j=j, N=N, t0=t0: e.tensor_tensor(out=sqt[:, j, 0:N], in0=cvb[:, j, t0:t0 + N], in1=mean[:, 0:N], op=ALU.subtract),
                              reads=[kcvb + str(j), kmean], writes=[ksqt + str(j)])
                        P.add("dve", lambda e, j=j, N=N: e.tensor_tensor(out=sqt[:, j, 0:N], in0=sqt[:, j, 0:N], in1=rst[:, 0:N], op=ALU.mult),
                              reads=[ksqt + str(j), krst], writes=[ksqt + str(j)])
                        P.add("act", lambda e, j=j, N=N: e.activation(out=yo[:, j, 0:N], in_=sqt[:, j, 0:N], func=AF.Silu, scale=self.vec("lng", j), bias=self.vec("lnb", j)),
                              reads=[ksqt + str(j), "vecs"], writes=[kyo + str(j)])
                    self.dma(self.mixT[1024:2048, col + t0:col + t0 + N].rearrange("(ch p) n -> p ch n", p=128), yo[:, :, 0:N], [kyo + str(j) for j in range(8)], ["mixT"])
        if not merged:
            P.flush()
        yield

    def stage_D(self, layer):
        c = self.cfg
        P, A = self.P, self.A
        A.reset()
        self.slabs = None
        self.cast_some(5 if layer == 0 else 99)
        KC, TB = c.KC, c.TB
        FC = c.DFF // 128 if layer == 0 else c.EFF // 128 // 4
        xb, kx = A.alloc((KC, TB), F32)
        mx, kmx = A.alloc((16, TB), BF16)
        sqb, ksq = A.alloc((KC, TB), BF16)
        f, kf = A.alloc((KC, TB), BF16)
        rst, krst = A.alloc((TB,), F32)
        tmps = [A.alloc((TB,), F32) for _ in range(3 if layer == 0 else 2)]
        hid, khid = A.alloc((FC, TB), BF16)
        wout = "wout%d" % layer
        if layer == 1:
            NT = TB // 128
            acc, kacc = A.alloc((KC, TB), F32)
            lg, klg = A.alloc((TB,), F32)
            lt, klt = A.alloc((NT, 8), F32)
            mx8, kmx8 = A.alloc((NT, 8), F32)
            ng, kng = A.alloc((NT,), F32)
            msk, kmsk = A.alloc((NT, 8), F32)
            den, kden = A.alloc((NT,), F32)
            GT, kGT = A.alloc((TB,), F32)
            gbc, kgbc = A.alloc((TB,), F32)
            selm, _ = A.alloc((8, 128), F32)
            self.dma(selm[0:8], self.selm_in, [], ["selm"])
            self.ps_lo = 1
            self.psn = 1
            pr, kpr = self.ps[0], "ps0"
        for (b, col0, N, is_ctx, pos0) in self.blocks():
            if layer == 1 and is_ctx:
                continue
            mi = c.NB if is_ctx else b
            self.load_x(layer, b, col0, N, is_ctx, pos0, xb, kx)
            self.dma(mx[:, :, 0:N], self.mixT[:, col0:col0 + N].rearrange("(ch p) n -> p ch n", p=128), ["mixT"], [kmx])

            def ev1(ch, ps_ap, pk, rows, N=N, mi=mi):
                P.add("dve", lambda e: e.scalar_tensor_tensor(out=xb[:, ch, 0:N], in0=ps_ap, scalar=self.mod[:, 2 * KC + ch, mi:mi + 1], in1=xb[:, ch, 0:N], op0=ALU.mult, op1=ALU.add),
                      reads=[pk, kx, "mod"], writes=[kx])
            self.gemm_fm(wout, 2048, 0, c.D, lambda kc, N=N: mx[:, kc, 0:N], [kmx], N, ev1)
            self.norm_mod(xb, kx, N, 1, mi, f, kf, sqb, ksq, rst, krst, tmps, router=((pr, kpr) if layer == 1 else None))
            fkeys = [kf + "_%d" % kc for kc in range(KC)]
            if layer == 1:
                self.moe_block(b, col0, N, pos0, mi, xb, kx, f, fkeys, hid, khid, FC, acc, kacc, lg, klg, lt, klt, mx8, kmx8, ng, kng, msk, kmsk, den, kden, GT, kGT, gbc, kgbc, selm, pr, kpr)
            if layer == 0:
                def evg(ch, ps_ap, pk, rows, N=N):
                    P.add("act", lambda e: e.activation(out=hid[:, ch, 0:N], in_=ps_ap, func=AF.Silu), reads=[pk], writes=[khid + str(ch)])

                def evu(ch, ps_ap, pk, rows, N=N):
                    P.add("dve", lambda e: e.tensor_tensor(out=hid[:, ch, 0:N], in0=ps_ap, in1=hid[:, ch, 0:N], op=ALU.mult), reads=[pk, khid + str(ch)], writes=[khid + str(ch)])
                self.gemm_fm("dg", c.D, 0, c.DFF, lambda kc, N=N: f[:, kc, 0:N], fkeys, N, evg)
                self.gemm_fm("du", c.D, 0, c.DFF, lambda kc, N=N: f[:, kc, 0:N], fkeys, N, evu)

                def evd(ch, ps_ap, pk, rows, N=N, mi=mi):
                    P.add("dve", lambda e: e.scalar_tensor_tensor(out=xb[:, ch, 0:N], in0=ps_ap, scalar=self.mod[:, 5 * KC + ch, mi:mi + 1], in1=xb[:, ch, 0:N], op0=ALU.mult, op1=ALU.add),
                          reads=[pk, kx, "mod"], writes=[kx])
                self.gemm_fm("dd", c.DFF, 0, c.D, lambda kc, N=N: hid[:, kc, 0:N], [khid + str(i) for i in range(FC)], N, evd)
                self.dma(self.xs[:, col0:col0 + N].rearrange("(kc p) n -> p kc n", p=128), xb[:, :, 0:N], [kx], ["xs"])
        self.ps_lo = 0
        P.flush()

    def moe_block(self, b, col0, N, pos0, mi, xb, kx, f, fkeys, hid, khid, FC, acc, kacc, lg, klg, lt, klt, mx8, kmx8, ng, kng, msk, kmsk, den, kden, GT, kGT, gbc, kgbc, selm, pr, kpr):
        c = self.cfg
        P = self.P
        KC = c.KC
        NT = (N + 127) // 128
        X = mybir.AxisListType.X
        P.add("act", lambda e: e.activation(out=lg[0:8, 0:N], in_=pr[0:8, 0:N], func=AF.Copy), reads=[kpr], writes=[klg])
        for tt in range(NT):
            tn = min(128, N - tt * 128)
            pT, kT_ = self.psum()
            P.add("pe", lambda e, pT=pT, tt=tt, tn=tn: e.transpose(pT[0:tn, 0:8], lg[0:8, tt * 128:tt * 128 + tn], self.matsf[0:8, 2, 0:8]), reads=[klg, "matsf"], writes=[kT_])
            P.add("act", lambda e, pT=pT, tt=tt, tn=tn: e.activation(out=lt[0:tn, tt, :], in_=pT[0:tn, 0:8], func=AF.Copy), reads=[kT_], writes=[klt])
            P.add("dve", lambda e, tt=tt, tn=tn: e.max(out=mx8[0:tn, tt, :], in_=lt[0:tn, tt, :]), reads=[klt], writes=[kmx8])
            P.add("dve", lambda e, tt=tt, tn=tn: e.tensor_scalar(out=ng[0:tn, tt:tt + 1], in0=mx8[0:tn, tt, 0:1], scalar1=-1.0, scalar2=None, op0=ALU.mult), reads=[kmx8], writes=[kng])
            P.add("dve", lambda e, tt=tt, tn=tn: e.tensor_scalar(out=msk[0:tn, tt, :], in0=lt[0:tn, tt, :], scalar1=mx8[0:tn, tt, 1:2], scalar2=None, op0=ALU.is_ge), reads=[klt, kmx8], writes=[kmsk])
            P.add("act", lambda e, tt=tt, tn=tn: e.activation(out=lt[0:tn, tt, :], in_=lt[0:tn, tt, :], func=AF.Exp, bias=ng[0:tn, tt:tt + 1], scale=1.0), reads=[klt, kng, kmsk], writes=[klt])
            P.add("dve", lambda e, tt=tt, tn=tn: e.tensor_tensor(out=msk[0:tn, tt, :], in0=msk[0:tn, tt, :], in1=lt[0:tn, tt, :], op=ALU.mult), reads=[klt, kmsk], writes=[kmsk])
            P.add("dve", lambda e, tt=tt, tn=tn: e.reduce_sum(out=den[0:tn, tt:tt + 1], in_=msk[0:tn, tt, :], axis=X), reads=[kmsk], writes=[kden])
            P.add("dve", lambda e, tt=tt, tn=tn: e.reciprocal(out=den[0:tn, tt:tt + 1], in_=den[0:tn, tt:tt + 1]), reads=[kden], writes=[kden])
            P.add("dve", lambda e, tt=tt, tn=tn: e.tensor_scalar(out=msk[0:tn, tt, :], in0=msk[0:tn, tt, :], scalar1=den[0:tn, tt:tt + 1], scalar2=None, op0=ALU.mult), reads=[kmsk, kden], writes=[kmsk])
            pG, kG = self.psum()
            P.add("pe", lambda e, pG=pG, tt=tt, tn=tn: e.transpose(pG[0:8, 0:tn], msk[0:tn, tt, :], self.matsf[0:tn, 2, 0:tn]), reads=[kmsk, "matsf"], writes=[kG])
            P.add("act", lambda e, pG=pG, tt=tt, tn=tn: e.activation(out=GT[0:8, tt * 128:tt * 128 + tn], in_=pG[0:8, 0:tn], func=AF.Copy), reads=[kG], writes=[kGT])
        NH = 4
        FCq = FC
        first = True
        for ex in range(c.E):
            pg, kg = self.psum()
            P.add("pe", lambda e, pg=pg, ex=ex: e.matmul(pg[:, 0:N], lhsT=selm[0:8, ex, :], rhs=GT[0:8, 0:N], start=True, stop=True), reads=[kGT, "selm"], writes=[kg])
            P.add("act", lambda e, pg=pg: e.activation(out=gbc[:, 0:N], in_=pg[:, 0:N], func=AF.Copy), reads=[kg], writes=[kgbc])
            for hq in range(NH):
                f0 = hq * FCq * 128

                def evg(ch, ps_ap, pk, rows, f0=f0):
                    cl = ch - f0 // 128
                    P.add("act", lambda e: e.activation(out=hid[:, cl, 0:N], in_=ps_ap, func=AF.Silu), reads=[pk], writes=[khid + str(cl)])

                def evu(ch, ps_ap, pk, rows, f0=f0):
                    cl = ch - f0 // 128
                    P.add("dve", lambda e: e.tensor_tensor(out=hid[:, cl, 0:N], in0=ps_ap, in1=hid[:, cl, 0:N], op=ALU.mult), reads=[pk, khid + str(cl)], writes=[khid + str(cl)])
                self.gemm_fm("eg%d" % ex, c.D, f0, f0 + FCq * 128, lambda kc: f[:, kc, 0:N], fkeys, N, evg)
                self.gemm_fm("eu%d" % ex, c.D, f0, f0 + FCq * 128, lambda kc: f[:, kc, 0:N], fkeys, N, evu)

                def evd(ch, ps_ap, pk, rows, first=first):
                    if first:
                        P.add("dve", lambda e: e.tensor_tensor(out=acc[:, ch, 0:N], in0=ps_ap, in1=gbc[:, 0:N], op=ALU.mult), reads=[pk, kgbc], writes=[kacc + str(ch)])
                    else:
                        P.add("dve", lambda e: e.tensor_tensor(out=lg[:, 0:N], in0=ps_ap, in1=gbc[:, 0:N], op=ALU.mult), reads=[pk, kgbc, klg], writes=[klg])
                        P.add("pool", lambda e: e.tensor_tensor(out=acc[:, ch, 0:N], in0=acc[:, ch, 0:N], in1=lg[:, 0:N], op=ALU.add), reads=[klg, kacc + str(ch)], writes=[kacc + str(ch)])
                self.gemm_fm("ed%d" % ex, FCq * 128, 0, c.D, lambda kc: hid[:, kc, 0:N], [khid + str(i) for i in range(FCq)], N, evd, krow0=f0)
                first = False
        for ch in range(KC):
            P.add("dve", lambda e, ch=ch: e.scalar_tensor_tensor(out=xb[:, ch, 0:N], in0=acc[:, ch, 0:N], scalar=self.mod[:, 5 * KC + ch, mi:mi + 1], in1=xb[:, ch, 0:N], op0=ALU.mult, op1=ALU.add),
                  reads=[kacc + str(ch), kx, "mod"], writes=[kx])
        self.dma(self.outT[b][:, pos0:pos0 + N].rearrange("(kc p) n -> p kc n", p=128), xb[:, :, 0:N], [kx], ["outT"])

    def stage_L1A(self):
        c = self.cfg
        P, A = self.P, self.A
        A.reset()
        self.slabs = None
        self.cast_some(4)
        KC, TB = c.KC, c.TB
        xb, kx = A.alloc((KC, TB), F32)
        sqb, ksq = A.alloc((KC, TB), BF16)
        u, ku = A.alloc((KC, TB), BF16)
        rst, krst = A.alloc((TB,), F32)
        tmps = [A.alloc((TB,), F32) for _ in range(3)]
        qr, kqr = A.alloc((4, TB), F32)
        qsq, kqsq = A.alloc((4, TB), BF16)
        qo, kqo = A.alloc((4, TB), BF16)
        rts = [A.alloc((TB,), F32) for _ in range(4)]
        vb, kvb = A.alloc((4, 512), BF16)
        rope, _ = A.alloc((2, c.TOKB), F32)
        self.dma(rope, self.rope_in[:, 2:4, :], [], ["rope"])
        cosT, sinT = rope[:, 0, :], rope[:, 1, :]
        g = self.gsc
        P.add("dve", lambda e: e.tensor_scalar(out=g[:, 10:11], in0=self.vec("sqg"), scalar1=1.0, scalar2=None, op0=ALU.mult), reads=["vecs"], writes=["gsc"])
        P.add("dve", lambda e: e.tensor_scalar(out=g[:, 11:12], in0=self.vec("skg"), scalar1=float(128 ** 0.5), scalar2=None, op0=ALU.mult), reads=["vecs"], writes=["gsc"])
        for (b, col0, N, is_ctx, pos0) in self.blocks():
            mi = c.NB if is_ctx else b
            self.load_x(1, b, col0, N, is_ctx, pos0, xb, kx)
            self.norm_mod(xb, kx, N, 0, mi, u, ku, sqb, ksq, rst, krst, tmps)
            ukeys = [ku + "_%d" % kc for kc in range(KC)]

            def evac(ch, ps_ap, pk, rows, N=N, col0=col0, pos0=pos0):
                if ch < 16:
                    i = ch % 2
                    gi = 10 if ch < 12 else 11
                    P.add("act", lambda e: e.activation(out=qr[:, i, 0:N], in_=ps_ap, func=AF.Copy), reads=[pk], writes=[kqr + str(i)])
                    P.add("act", lambda e: e.activation(out=qsq[:, i, 0:N], in_=ps_ap, func=AF.Square), reads=[pk], writes=[kqsq + str(i)])
                    pb, pk2 = self.ssq([(0, qsq[:, i, 0:N])], N, [kqsq + str(i)])
                    tp, tk = tmps[i]
                    self.rstd_from(pb, pk2, N, 128 * EPS, tp[:, 0:N], tk)
                    src = qr[:, i, 0:N]
                    P.add("dve", lambda e: e.scalar_tensor_tensor(out=src, in0=src, scalar=g[:, gi:gi + 1], in1=tp[:, 0:N], op0=ALU.mult, op1=ALU.mult),
                          reads=[kqr + str(i), tk, "gsc"], writes=[kqr + str(i)])
                    pb3, pk3 = self.psum()
                    P.add("pe", lambda e: e.matmul(pb3[:, 0:N], lhsT=self.matsf[:, 1, :], rhs=src, start=True, stop=True), reads=[kqr + str(i), "matsf"], writes=[pk3])
                    P.add("dve", lambda e: e.tensor_tensor(out=tp[:, 0:N], in0=pb3[:, 0:N], in1=sinT[:, pos0:pos0 + N], op=ALU.mult), reads=[pk3, "rope", tk], writes=[tk])
                    P.add("dve", lambda e: e.tensor_tensor(out=src, in0=src, in1=cosT[:, pos0:pos0 + N], op=ALU.mult), reads=[kqr + str(i), "rope", pk3], writes=[kqr + str(i)])
                    P.add("dve", lambda e: e.tensor_tensor(out=qo[:, i, 0:N], in0=src, in1=tp[:, 0:N], op=ALU.add), reads=[kqr + str(i), tk], writes=[kqo + str(i)])
                    dst = self.qT[ch * 128:(ch + 1) * 128, col0:col0 + N] if ch < 12 else self.kT[(ch - 12) * 128:(ch - 11) * 128, col0:col0 + N]
                    self.dma(dst, qo[:, i, 0:N], [kqo + str(i)], ["qT" if ch < 12 else "kT"])
                else:
                    i = ch % 2
                    P.add("act", lambda e: e.activation(out=qo[:, i, 0:N], in_=ps_ap, func=AF.Copy), reads=[pk], writes=[kqo + str(i)])
                    self.dma(self.fT[(ch - 20) * 128:(ch - 19) * 128, col0:col0 + N], qo[:, i, 0:N], [kqo + str(i)], ["fT"])
            rhs = lambda kc, N=N: u[:, kc, 0:N]

            def evgrp(items, N=N, col0=col0, pos0=pos0):
                for i, (ch, ps_ap, pk) in enumerate(items):
                    P.add("act", lambda e, i=i, ps_ap=ps_ap: e.activation(out=qr[:, i, 0:N], in_=ps_ap, func=AF.Copy), reads=[pk], writes=[kqr + str(i)])
                    P.add("act", lambda e, i=i, ps_ap=ps_ap: e.activation(out=qsq[:, i, 0:N], in_=ps_ap, func=AF.Square), reads=[pk], writes=[kqsq + str(i)])
                pbs = [self.ssq([(0, qsq[:, i, 0:N])], N, [kqsq + str(i)]) for i in range(len(items))]
                for i in range(len(items)):
                    tp, tk = rts[i]
                    P.add("dve", lambda e, i=i, tp=tp: e.tensor_scalar(out=tp[:, 0:N], in0=pbs[i][0][:, 0:N], scalar1=float(128 * EPS), scalar2=None, op0=ALU.add), reads=[pbs[i][1]], writes=[tk])
                for i in range(len(items)):
                    tp, tk = rts[i]
                    P.add("act", lambda e, tp=tp: e.activation(out=tp[:, 0:N], in_=tp[:, 0:N], func=AF.Ln), reads=[tk], writes=[tk])
                for i in range(len(items)):
                    tp, tk = rts[i]
                    P.add("act", lambda e, tp=tp: e.activation(out=tp[:, 0:N], in_=tp[:, 0:N], func=AF.Exp, scale=-0.5), reads=[tk], writes=[tk])
                for i, (ch, ps_ap, pk) in enumerate(items):
                    tp, tk = rts[i]
                    gi = 10 if ch < 12 else 11
                    P.add("dve", lambda e, i=i, tp=tp, gi=gi: e.scalar_tensor_tensor(out=qr[:, i, 0:N], in0=qr[:, i, 0:N], scalar=g[:, gi:gi + 1], in1=tp[:, 0:N], op0=ALU.mult, op1=ALU.mult),
                          reads=[kqr + str(i), tk, "gsc"], writes=[kqr + str(i)])
                pb3 = []
                for i in range(len(items)):
                    p3, k3 = self.psum()
                    pb3.append((p3, k3))
                    P.add("pe", lambda e, i=i, p3=p3: e.matmul(p3[:, 0:N], lhsT=self.matsf[:, 1, :], rhs=qr[:, i, 0:N], start=True, stop=True), reads=[kqr + str(i), "matsf"], writes=[k3])
                for i in range(len(items)):
                    tp, tk = rts[i]
                    P.add("dve", lambda e, i=i, tp=tp: e.tensor_tensor(out=tp[:, 0:N], in0=pb3[i][0][:, 0:N], in1=sinT[:, pos0:pos0 + N], op=ALU.mult), reads=[pb3[i][1], "rope", tk], writes=[tk])
                    P.add("dve", lambda e, i=i: e.tensor_tensor(out=qr[:, i, 0:N], in0=qr[:, i, 0:N], in1=cosT[:, pos0:pos0 + N], op=ALU.mult), reads=[kqr + str(i), "rope", pb3[i][1]], writes=[kqr + str(i)])
                for i, (ch, ps_ap, pk) in enumerate(items):
                    tp, tk = rts[i]
                    P.add("dve", lambda e, i=i, tp=tp: e.tensor_tensor(out=qo[:, i, 0:N], in0=qr[:, i, 0:N], in1=tp[:, 0:N], op=ALU.add), reads=[kqr + str(i), tk], writes=[kqo + str(i)])
                    dst = self.qT[ch * 128:(ch + 1) * 128, col0:col0 + N] if ch < 12 else self.kT[(ch - 12) * 128:(ch - 11) * 128, col0:col0 + N]
                    self.dma(dst, qo[:, i, 0:N], [kqo + str(i)], ["qT" if ch < 12 else "kT"])
            if not is_ctx:
                self.gemm_fm("win1", c.D, 0, 2048, rhs, ukeys, N, evac, evac_group=evgrp)
                self.gemm_fm("win1", c.D, 2560, 3072, rhs, ukeys, N, evac)
            else:
                self.gemm_fm("win1", c.D, 1536, 2048, rhs, ukeys, N, evac, evac_group=evgrp)

            def evv(g0, t0, tn, ps_ap, pk, gc, col0=col0):
                ti = t0 // 128
                P.add("act", lambda e: e.activation(out=vb[0:tn, ti, 0:gc], in_=ps_ap, func=AF.Copy), reads=[pk], writes=[kvb + str(ti)])
                self.dma(self.Vt[col0 + t0:col0 + t0 + tn, 0:512], vb[0:tn, ti, :], [kvb + str(ti)], ["Vt"])
            self.gemm_tm("win1", c.D, 2048, 2560, u, ukeys, N, evv)
        P.flush()

    def stage_L1B(self, merged=False):
        c = self.cfg
        P, A = self.P, self.A
        if not merged:
            A.reset()
            self.slabs = None
        self.cast_some(3)
        NKC = c.TOKB // 128
        NQ = c.S // 128
        kn, kkn = A.alloc((c.TOKB,), BF16)
        vh, kvh = A.alloc((NKC, 128), BF16)
        qh, kqh = A.alloc((c.S,), BF16)
        pts = [A.alloc((128,), BF16) for _ in range(4)]
        wm, _ = A.alloc((2, 128), BF16)
        esink, _ = A.alloc((12,), F32)
        rden, krden = A.alloc((128,), F32)
        ob, kob = A.alloc((c.S,), BF16)
        self.dma(wm, self.wmask_in, [], ["wm"])
        P.add("act", lambda e: e.activation(out=esink, in_=self.vec("sink", 0, 12), func=AF.Exp), reads=["vecs"], writes=["esink"])
        self.ps_lo = 2
        self.psn = 2
        pO, kO, pD, kD = self.ps[0], "ps0", self.ps[1], "ps1"
        for b in range(c.NB):
            cb = b * c.TOKB
            for n in range(4):
                self.dma(kn, self.kT[n * 128:(n + 1) * 128, cb:cb + c.TOKB], ["kT"], [kkn])
                self.dma(vh, self.Vt[cb:cb + c.TOKB, n * 128:(n + 1) * 128].rearrange("(kc p) d -> p kc d", p=128), ["Vt"], [kvh])
                for gq in range(3):
                    yield
                    hq = n * 3 + gq
                    self.dma(qh, self.qT[hq * 128:(hq + 1) * 128, cb:cb + c.S], ["qT"], [kqh])
                    for j in range(NQ):
                        kcs = []
                        if j > 0:
                            kcs.append((j - 1, 0))
                        kcs.append((j, None))
                        if j < NQ - 1:
                            kcs.append((j + 1, 1))
                        kcs += [(kc, None) for kc in range(NQ, NKC)]
                        for i, (kc, mk) in enumerate(kcs):
                            pS, kS = self.psum()
                            pt, kpt = pts[i % 4]
                            P.add("pe", lambda e, pS=pS, kc=kc, j=j: e.matmul(pS[:, 0:128], lhsT=kn[:, kc * 128:(kc + 1) * 128], rhs=qh[:, j * 128:(j + 1) * 128], start=True, stop=True),
                                  reads=[kkn, kqh], writes=[kS])
                            P.add("act", lambda e, pS=pS, pt=pt: e.activation(out=pt, in_=pS[:, 0:128], func=AF.Exp), reads=[kS], writes=[kpt])
                            if mk is not None:
                                P.add("dve", lambda e, pt=pt, mk=mk: e.tensor_tensor(out=pt, in0=pt, in1=wm[:, mk, :], op=ALU.mult), reads=[kpt, "wm"], writes=[kpt])

                            def mmO(e, pt=pt, kc=kc, first=(i == 0), last=(i == len(kcs) - 1)):
                                e.matmul(pO[:, 0:128], lhsT=vh[:, kc, :], rhs=pt, start=first, stop=last)
                                return e.matmul(pD[:, 0:128], lhsT=self.mats[:, 0, :], rhs=pt, start=first, stop=last)
                            P.add("pe", mmO, reads=[kvh, kpt, "mats"], writes=[kO, kD])
                        P.add("dve", lambda e, hq=hq: e.tensor_scalar(out=rden, in0=pD[:, 0:128], scalar1=esink[:, hq:hq + 1], scalar2=None, op0=ALU.add), reads=[kD, "esink"], writes=[krden])
                        P.add("dve", lambda e: e.reciprocal(out=rden, in_=rden), reads=[krden], writes=[krden])
                        P.add("dve", lambda e, j=j: e.tensor_tensor(out=ob[:, j * 128:(j + 1) * 128], in0=pO[:, 0:128], in1=rden, op=ALU.mult), reads=[kO, krden], writes=[kob])
                    self.dma(self.mixT[hq * 128:(hq + 1) * 128, cb:cb + c.S], ob, [kob], ["mixT"])
        if not merged:
            self.ps_lo = 0
            P.flush()
        yield

    def stage_L1C(self, merged=False):
        c = self.cfg
        P, A = self.P, self.A
        if not merged:
            A.reset()
            self.slabs = None
        self.cast_some(3)
        S = c.S
        NT = S // 128
        NBK = min(512, S)
        fin, kfin = A.alloc((S,), BF16)
        dc, _ = A.alloc((256,), BF16)
        ab, kab = A.alloc((NT, 256), BF16)
        cs = [A.alloc((NT, NBK), BF16) for _ in range(2)]
        yo, kyo = A.alloc((NBK,), BF16)
        self.dma(dc, self.dftc_in, [], ["dc"])
        scale = float(1.0 / np.sqrt(S * 128.0))
        for b in range(c.NB):
            cb = b * c.TOKB
            for gi in range(4):
                yield
                self.dma(fin, self.fT[gi * 128:(gi + 1) * 128, cb:cb + S], ["fT"], [kfin])
                for tt in range(NT):
                    pb, pk = self.psum()
                    P.add("pe", lambda e, pb=pb, tt=tt: e.matmul(pb[:, 0:256], lhsT=fin[:, tt * 128:(tt + 1) * 128], rhs=dc, start=True, stop=True), reads=[kfin, "dc"], writes=[pk])
                    P.add("act", lambda e, pb=pb, tt=tt: e.activation(out=ab[:, tt, :], in_=pb[:, 0:256], func=AF.Copy), reads=[pk], writes=[kab])
                for t0 in range(0, S, NBK):
                    yield
                    for ci in range(2):
                        self.dma(cs[ci][0], self.dftT_in[ci][:, t0:t0 + NBK].rearrange("(tt p) n -> p tt n", p=128), [], [cs[ci][1]])
                    pb, pk = self.psum()

                    def mm(e, pb=pb):
                        m = None
                        for tt in range(NT):
                            e.matmul(pb[:, 0:NBK], lhsT=ab[:, tt, 0:128], rhs=cs[0][0][:, tt, :], start=(tt == 0), stop=False)
                            m = e.matmul(pb[:, 0:NBK], lhsT=ab[:, tt, 128:256], rhs=cs[1][0][:, tt, :], start=False, stop=(tt == NT - 1))
                        return m
                    P.add("pe", mm, reads=[kab, cs[0][1], cs[1][1]], writes=[pk])
                    P.add("act", lambda e, pb=pb: e.activation(out=yo, in_=pb[:, 0:NBK], func=AF.Copy, scale=scale), reads=[pk], writes=[kyo])
                    self.dma(self.mixT[1536 + gi * 128:1536 + (gi + 1) * 128, cb + t0:cb + t0 + NBK], yo, [kyo], ["mixT"])
        if not merged:
            P.flush()
        yield


def run_merged(B, f1, f2):
    B.A.reset()
    B.slabs = None
    gens = [f1(merged=True), f2(merged=True)]
    while gens:
        for g in list(gens):
            try:
                next(g)
            except StopIteration:
                gens.remove(g)
    B.ps_lo = 0
    B.P.flush()


def build_all(cfg):
    B = Builder(cfg, layers=(0, 1))
    B.stage_prep()
    B.stage_mod(0)
    B.stage_L0A()
    run_merged(B, B.stage_L0B, B.stage_L0C)
    B.stage_D(0)
    B.stage_mod(1)
    B.stage_L1A()
    run_merged(B, B.stage_L1B, B.stage_L1C)
    B.stage_D(1)
    B.P.final_wait()
    return B

BF = ml_dtypes.bfloat16

def chunks(v):
    return np.ascontiguousarray(v.reshape(-1, 128).T)

def rope_tables(S, L, rot_dim, rep):
    rows = S // 64
    row = np.repeat(np.arange(rows, dtype=np.float32), 64)
    col = np.tile(np.arange(64, dtype=np.float32), rows)
    half = rot_dim // 2
    inv = (10000.0 ** (-np.arange(0, half, 2, dtype=np.float32) / half)).astype(np.float32)
    nf = len(inv)
    ang_r = row[:, None] * inv[None, :]
    ang_c = col[:, None] * inv[None, :]
    cos = np.concatenate([np.cos(ang_r), np.cos(ang_r), np.cos(ang_c), np.cos(ang_c)], 1).T
    sin = np.concatenate([np.sin(ang_r), np.sin(ang_r), np.sin(ang_c), np.sin(ang_c)], 1).T
    cos = np.concatenate([cos, np.ones((rot_dim, L), np.float32)], 1)
    sin = np.concatenate([sin, np.zeros((rot_dim, L), np.float32)], 1)
    return np.tile(cos, (rep, 1)).astype(np.float32), np.tile(sin, (rep, 1)).astype(np.float32)

def perm_matrix(rot_dim, rep):
    nf = rot_dim // 4
    Pm = np.zeros((rot_dim, rot_dim), np.float32)
    for base in (0, 2 * nf):
        for i in range(nf):
            Pm[base + i, base + nf + i] = -1.0
            Pm[base + nf + i, base + i] = 1.0
    full = np.zeros((128, 128), np.float32)
    for r in range(rep):
        full[r * rot_dim:(r + 1) * rot_dim, r * rot_dim:(r + 1) * rot_dim] = Pm
    return np.ascontiguousarray(full.T)

def prep_common(cfg, inp):
    D = cfg.D
    out = {}
    w0 = inp["even_w_in"][0]
    out["w_win0"] = np.ascontiguousarray(np.concatenate([w0[:, 0:768], w0[:, 832:2880], w0[:, 768:832], w0[:, 768:832]], 1))
    wq = inp["mla_w_q_b"][0].reshape(512, 8, 192)
    out["w_wqb"] = np.ascontiguousarray(np.concatenate([wq[:, :, :128].reshape(512, 1024), wq[:, :, 128:].reshape(512, 512)], 1))
    wk = inp["mla_w_kv_b"][0].reshape(256, 8, 256)
    out["w_wkvb"] = np.ascontiguousarray(np.concatenate([wk[:, :, :128].reshape(256, 1024), wk[:, :, 128:].reshape(256, 1024)], 1))
    out["w_ada0"] = inp["ada_w"][0]; out["w_ada1"] = inp["ada_w"][1]
    out["w_wout0"] = inp["even_w_out"][0]
    out["w_dg"] = inp["dense_w_gate"][0]; out["w_du"] = inp["dense_w_up"][0]; out["w_dd"] = inp["dense_w_down"][0]
    out["w_win1"] = inp["odd_w_in"][0]; out["w_wout1"] = inp["odd_w_out"][0]
    for e in range(cfg.E):
        out["w_eg%d" % e] = inp["expert_w_gate"][0, e]; out["w_eu%d" % e] = inp["expert_w_up"][0, e]; out["w_ed%d" % e] = inp["expert_w_down"][0, e]
    off, nv = vec_layout(cfg)
    V = np.zeros((128, nv), np.float32)
    def put(name, arr):
        o, w = off[name]; assert arr.shape == (128, w), (name, arr.shape, w); V[:, o:o + w] = arr
    put("mixg0", chunks(inp["mix_norm_g"][0])); put("mixg1", chunks(inp["mix_norm_g"][1]))
    put("ffng0", chunks(inp["ffn_norm_g"][0])); put("ffng1", chunks(inp["ffn_norm_g"][1]))
    put("adab0", chunks(inp["ada_b"][0])); put("adab1", chunks(inp["ada_b"][1]))
    put("qag", chunks(inp["mla_q_a_norm_g"][0])); put("kvag", chunks(inp["mla_kv_a_norm_g"][0]))
    qg = inp["mla_q_norm_g"][0]; kg = inp["mla_k_norm_g"][0]
    put("qng_n", qg[:128, None]); put("qng_r", np.tile(qg[128:], 2)[:, None])
    put("kng_n", kg[:128, None]); put("kng_r", np.tile(kg[128:], 2)[:, None])
    dw = inp["conv_dw_w"][0]
    put("dww", np.ascontiguousarray(dw.T.reshape(8, 128, 31).transpose(1, 0, 2)).reshape(128, 8 * 31))
    put("dwb", chunks(inp["conv_dw_b"][0])); put("lng", chunks(inp["conv_ln_g"][0])); put("lnb", chunks(inp["conv_ln_b"][0]))
    put("sqg", inp["swa_q_norm_g"][0][:, None]); put("skg", inp["swa_k_norm_g"][0][:, None])
    put("sink", np.tile(inp["swa_sink"][0][None, :], (128, 1)))
    rw = inp["router_w"][0]
    put("rw", np.ascontiguousarray(rw.reshape(cfg.KC, 128, 8).transpose(1, 0, 2)).reshape(128, cfg.KC * 8))
    out["vecs"] = V
    M = np.zeros((128, 6, 128), np.float32)
    M[:, 0, :] = 1.0
    M[0:64, 1, :] = 1.0
    M[64:128, 2, :] = 1.0
    M[:, 3, 0:64] = 1.0
    M[:, 4, 64:128] = 1.0
    M[0:64, 5, 0:64] = 1.0; M[64:128, 5, 64:128] = 1.0
    out["mats"] = M.astype(BF)
    MF = np.zeros((128, 4, 128), np.float32)
    MF[:, 0, :] = perm_matrix(64, 2)
    MF[:, 1, :] = perm_matrix(128, 1)
    MF[:, 2, :] = np.eye(128, dtype=np.float32)
    MF[:, 3, :] = 1.0
    out["matsf"] = MF
    c0, s0 = rope_tables(cfg.S, cfg.L, 64, 2)
    c1, s1 = rope_tables(cfg.S, cfg.L, 128, 1)
    out["rope"] = np.ascontiguousarray(np.stack([c0, s0, c1, s1], 1))
    wm = np.zeros((128, 2, 128), np.float32)
    kk = np.arange(128)[:, None]; qq = np.arange(128)[None, :]
    wm[:, 0, :] = (qq <= kk); wm[:, 1, :] = (kk <= qq)
    out["wmask"] = wm.astype(BF)
    sel = np.zeros((8, 8, 128), np.float32)
    for e_ in range(8): sel[e_, e_, :] = 1.0
    out["selm"] = sel
    cc = np.arange(128)
    ang = 2 * np.pi * np.outer(cc, cc) / 128.0
    out["dftc"] = np.concatenate([np.cos(ang), -np.sin(ang)], 1).astype(np.float32).astype(BF)
    tt = np.arange(cfg.S, dtype=np.int64)
    angT = 2 * np.pi * ((np.outer(tt, tt) % cfg.S).astype(np.float64)) / cfg.S
    out["dftT"] = np.stack([np.cos(angT), np.sin(angT)], 0).astype(np.float32).astype(BF)
    return out

def prep_core(cfg, inp, core):
    NB = cfg.NB
    bs = slice(core * NB, (core + 1) * NB)
    o = {}
    o["xT"] = np.ascontiguousarray(inp["x"][bs].transpose(0, 2, 1))
    o["cT"] = np.ascontiguousarray(inp["ctx"][bs].transpose(0, 2, 1))
    o["cvec"] = np.ascontiguousarray(np.concatenate([inp["c"][bs], inp["c_ctx"][None, :]], 0).T)
    return o


def kernel(**inputs):
    from concourse.bass_utils import run_bass_kernel_spmd
    inp = {k: np.asarray(v) for k, v in inputs.items()}
    cfg = Cfg(NB=2, S=2048, L=256, D=2048, DFF=5632, EFF=7168, E=8)
    NCORES = 8
    B = build_all(cfg)
    common = prep_common(cfg, inp)
    in_maps = []
    for c in range(NCORES):
        m = dict(common)
        m.update(prep_core(cfg, inp, c))
        in_maps.append(m)
    res = run_bass_kernel_spmd(B.nc, in_maps, core_ids=list(range(NCORES)))
    outs = [np.ascontiguousarray(np.asarray(r["outT"]).transpose(0, 2, 1)) for r in res.results]
    return np.concatenate(outs, axis=0).astype(np.float32)
```

```python
import numpy as np
import concourse.bass as bass
import concourse.mybir as mybir

F32 = mybir.dt.float32
BF16 = mybir.dt.bfloat16
AF = mybir.ActivationFunctionType
ALU = mybir.AluOpType
DT_SIZE = {F32: 4, BF16: 2}


class Prog:
    CE = ("pe", "act", "dve", "pool")
    DQ = ("sp", "pool")

    def __init__(self, nc, nslots=14):
        self.nc = nc
        self.ops = []
        self.csem = {e: nc.alloc_semaphore("c_" + e) for e in self.CE}
        self.ccount = {e: 0 for e in self.CE}
        self.dsem = {q: [nc.alloc_semaphore("d_%s%d" % (q, i)) for i in range(nslots)] for q in self.DQ}
        self.duse = {q: [0] * nslots for q in self.DQ}
        self.dnext = {q: 0 for q in self.DQ}
        self.nslots = nslots
        self.n_emitted = 0

    def add(self, eng, fn, reads=(), writes=(), dma=False):
        self.ops.append((eng, fn, tuple(reads), tuple(writes), dma))

    def flush(self):
        ops = self.ops
        self.ops = []
        if not ops:
            return
        n = len(ops)
        last_w = {}
        readers = {}
        deps = [None] * n
        signaled = [False] * n
        for i, (eng, fn, reads, writes, dma) in enumerate(ops):
            d = set()
            for k in reads:
                if k in last_w:
                    d.add(last_w[k])
            for k in writes:
                if k in last_w:
                    d.add(last_w[k])
                for r in readers.get(k, ()):
                    d.add(r)
            d.discard(i)
            dd = []
            for j in d:
                je, _, _, _, jd = ops[j]
                if (not jd) and je == "pe" and eng == "pe" and not dma:
                    continue
                dd.append(j)
                if not jd:
                    signaled[j] = True
            deps[i] = dd
            for k in writes:
                last_w[k] = i
                readers[k] = []
            for k in reads:
                readers.setdefault(k, []).append(i)
        token = [None] * n
        pre_wait = [None] * n
        ccount = dict(self.ccount)
        for i, (eng, fn, reads, writes, dma) in enumerate(ops):
            if dma:
                q = eng
                s = self.dnext[q]
                self.dnext[q] = (s + 1) % self.nslots
                prev = self.duse[q][s]
                self.duse[q][s] = prev + 1
                pre_wait[i] = (("d", q, s), 16 * prev)
                token[i] = (("d", q, s), 16 * (prev + 1))
            elif signaled[i]:
                ccount[eng] += 1
                token[i] = (("c", eng), ccount[eng])
        start_vals = dict(self.ccount)
        self.ccount = ccount
        base_known = {}
        for e in self.CE:
            base_known[("c", e)] = start_vals[e]
        prev_duse = {}
        per_eng = {e: [] for e in ("pe", "act", "dve", "pool", "sp")}
        for i, op in enumerate(ops):
            per_eng[op[0]].append(i)
        uses_this = {q: [0] * self.nslots for q in self.DQ}
        for i, op in enumerate(ops):
            if op[4]:
                (_, q, s), v = token[i]
                uses_this[q][s] += 1
        for q in self.DQ:
            for s in range(self.nslots):
                base_known[("d", q, s)] = 16 * (self.duse[q][s] - uses_this[q][s])

        def semh(key):
            return self.csem[key[1]] if key[0] == "c" else self.dsem[key[1]][key[2]]

        nc = self.nc
        first_flush = self.n_emitted == 0
        self.n_emitted += 1

        def emit_engine(eng_name, e):
            known = {}
            if not first_flush:
                for key, v in base_known.items():
                    if v > 0:
                        e.wait_ge(semh(key), v)
            known.update(base_known)
            for i in per_eng[eng_name]:
                _, fn, reads, writes, dma = ops[i]
                if pre_wait[i] is not None:
                    key, v = pre_wait[i]
                    if known.get(key, 0) < v:
                        e.wait_ge(semh(key), v)
                        known[key] = v
                for j in deps[i]:
                    key, v = token[j]
                    if known.get(key, 0) < v:
                        e.wait_ge(semh(key), v)
                        known[key] = v
                ins = fn(e)
                if token[i] is not None:
                    key, v = token[i]
                    ins.then_inc(semh(key), 16 if key[0] == "d" else 1)

        with nc.Block() as block:
            @block.tensor
            def _(e):
                emit_engine("pe", e)

            @block.scalar
            def _(e):
                emit_engine("act", e)

            @block.vector
            def _(e):
                emit_engine("dve", e)

            @block.gpsimd
            def _(e):
                emit_engine("pool", e)

            @block.sync
            def _(e):
                emit_engine("sp", e)

    def final_wait(self):
        nc = self.nc
        with nc.Block() as block:
            @block.sync
            def _(e):
                for en in self.CE:
                    if self.ccount[en] > 0:
                        e.wait_ge(self.csem[en], self.ccount[en])
                for q in self.DQ:
                    for s in range(self.nslots):
                        if self.duse[q][s] > 0:
                            e.wait_ge(self.dsem[q][s], 16 * self.duse[q][s])


class Arena:
    def __init__(self, nc, name, nbytes):
        self.t = nc.alloc_sbuf_tensor(name, [128, nbytes // 4], F32)
        self.cap = nbytes
        self.off = 0
        self.name = name
        self.n = 0

    def reset(self):
        self.off = 0

    def alloc(self, free_shape, dtype):
        sz = DT_SIZE[dtype]
        nel = int(np.prod(free_shape))
        nb = (nel * sz + 31) // 32 * 32
        assert self.off + nb <= self.cap, ("SBUF arena overflow", self.name, self.off, nb, self.cap)
        ap = self.t[:, self.off // 4:(self.off + nb) // 4]
        if dtype != F32:
            ap = ap.bitcast(dtype)
        ap = ap[:, 0:nel]
        if len(free_shape) == 2:
            ap = ap.rearrange("p (a b) -> p a b", a=free_shape[0])
        elif len(free_shape) == 3:
            ap = ap.rearrange("p (a b c) -> p a b c", a=free_shape[0], b=free_shape[1])
        self.off += nb
        self.n += 1
        return ap, "%s_%d_%d" % (self.name, self.off, self.n)


import ml_dtypes

EPS = 1e-6


class Cfg:
    def __init__(self, NB=2, S=2048, L=256, D=2048, DFF=5632, EFF=7168, E=8):
        self.NB, self.S, self.L, self.D, self.DFF, self.EFF, self.E = NB, S, L, D, DFF, EFF, E
        self.TOKB = S + L
        self.T = NB * self.TOKB
        self.KC = D // 128
        self.TB = min(512, S)


def vec_layout(cfg):
    KC = cfg.KC
    names = [("mixg0", KC), ("ffng0", KC), ("mixg1", KC), ("ffng1", KC),
             ("adab0", 6 * KC), ("adab1", 6 * KC),
             ("qag", 4), ("kvag", 2), ("qng_n", 1), ("qng_r", 1), ("kng_n", 1), ("kng_r", 1),
             ("dww", 8 * 31), ("dwb", 8), ("lng", 8), ("lnb", 8),
             ("sqg", 1), ("skg", 1), ("sink", 12), ("rw", KC * 8)]
    off = {}
    o = 0
    for n, w in names:
        off[n] = (o, w)
        o += w
    return off, o


class Builder:
    def __init__(self, cfg, layers=(0, 1)):
        self.cfg = cfg
        self.layers = layers
        nc = self.nc = bass.Bass("TRN2", target_bir_lowering=False)
        c = cfg
        self.P = Prog(nc)
        self.A = Arena(nc, "arena", 190 * 1024)
        self.Pers = Arena(nc, "pers", 12 * 1024)
        self.ps = [nc.alloc_psum_tensor("ps%d" % i, [128, 512], F32).ap() for i in range(8)]
        self.psn = 0
        self.voff, self.nv = vec_layout(cfg)
        D, KC = c.D, c.KC

        def ext(name, shape, dt=F32):
            return nc.dram_tensor(name, list(shape), dt, kind="ExternalInput").ap()

        def scr(name, shape, dt):
            return nc.dram_tensor(name, list(shape), dt).ap()

        self.xT_in = ext("xT", [c.NB, D, c.S])
        self.cT_in = ext("cT", [c.NB, D, c.L])
        self.cvec = ext("cvec", [D, c.NB + 1])
        self.vecs_in = ext("vecs", [128, self.nv])
        self.mats_in = ext("mats", [128, 6, 128], BF16)
        self.matsf_in = ext("matsf", [128, 4, 128])
        self.rope_in = ext("rope", [128, 4, c.TOKB])
        self.wmask_in = ext("wmask", [128, 2, 128], BF16)
        self.dftc_in = ext("dftc", [128, 256], BF16)
        self.dftT_in = ext("dftT", [2, c.S, c.S], BF16)
        self.selm_in = ext("selm", [8, 8, 128])
        self.outT = nc.dram_tensor("outT", [c.NB, D, c.S], F32, kind="ExternalOutput").ap()
        W = {}
        Wshapes = {"ada0": (D, 6 * D), "ada1": (D, 6 * D), "win0": (D, 2944), "wqb": (512, 1536), "wkvb": (256, 2048),
                   "wout0": (2048, D), "dg": (D, c.DFF), "du": (D, c.DFF), "dd": (c.DFF, D),
                   "win1": (D, 3072), "wout1": (2048, D)}
        for e in range(c.E):
            Wshapes["eg%d" % e] = (D, c.EFF)
            Wshapes["eu%d" % e] = (D, c.EFF)
            Wshapes["ed%d" % e] = (c.EFF, D)
        self.Wshapes = Wshapes
        self.Wext = {}
        self.Wbf = {}
        for k, shp in Wshapes.items():
            self.Wext[k] = ext("w_" + k, shp)
            self.Wbf[k] = scr("b_" + k, shp, BF16)
        T = c.T
        self.xs = scr("xs", [D, T], F32)
        self.hT = scr("hT", [1024, T], F32)
        self.qT = scr("qT", [1536, T], BF16)
        self.kT = scr("kT", [1536, T], BF16)
        self.Vt = scr("Vt", [T, 1024], BF16)
        self.mixT = scr("mixT", [2048, T], BF16)
        self.fT = scr("fT", [512, T], BF16)

    def psum(self):
        lo = getattr(self, "ps_lo", 0)
        if self.psn < lo:
            self.psn = lo
        i = self.psn
        self.psn = self.psn + 1
        if self.psn >= 8:
            self.psn = lo
        return self.ps[i], "ps%d" % i

    def dma(self, out, in_, reads, writes, q="sp"):
        self.P.add(q, lambda e: e.dma_start(out=out, in_=in_), reads=reads, writes=writes, dma=True)

    def vec(self, name, j=0, n=1):
        o, w = self.voff[name]
        return self.vecs[:, o + j:o + j + n]

    def stage_prep(self):
        c = self.cfg
        P = self.P
        self.deferred = []
        for k, shp in self.Wshapes.items():
            if k in ("ada1", "win1", "wout1") or k[0] == "e":
                if 1 not in self.layers:
                    continue
            if k[0] == "e" and k not in ("even",):
                self.deferred.append(k)
                continue
            self.cast_weight(k)
        self.vecs, kv = self.Pers.alloc((self.nv,), F32)
        self.mats, km = self.Pers.alloc((6, 128), BF16)
        self.matsf, kf = self.Pers.alloc((4, 128), F32)
        self.dma(self.vecs, self.vecs_in, [], ["vecs"])
        self.dma(self.mats, self.mats_in, [], ["mats"])
        self.dma(self.matsf, self.matsf_in, [], ["matsf"])
        self.mod, _ = self.Pers.alloc((6 * c.KC, c.NB + 1), F32)
        self.moda, _ = self.Pers.alloc((2 * c.KC, c.NB + 1), F32)
        self.gsc, _ = self.Pers.alloc((16,), F32)
        P.flush()

    def cast_weight(self, k):
        shp = self.Wshapes[k]
        rows = shp[0]
        step = max(128, (8 << 20) // (shp[1] * 4) // 128 * 128)
        for r0 in range(0, rows, step):
            r1 = min(rows, r0 + step)
            self.dma(self.Wbf[k][r0:r1, :], self.Wext[k][r0:r1, :], reads=[], writes=["W" + k], q="pool")

    def cast_some(self, n):
        for _ in range(n):
            if self.deferred:
                self.cast_weight(self.deferred.pop(0))

    def gemm_fm(self, wname, K, c0, c1, rhs, rhs_keys, N, evac, arena=None, krow0=0, evac_group=None):
        P = self.P
        Wd = self.Wbf[wname]
        KCn = K // 128
        if not hasattr(self, "slabs") or self.slabs is None:
            A = arena or self.A
            self.slabs = [A.alloc((16, 512), BF16) for _ in range(2)]
            self.slabn = 0
        for g0 in range(c0, c1, 512):
            gc = min(512, c1 - g0)
            nch = (gc + 127) // 128
            banks = [self.psum() for _ in range(nch)]
            for s0 in range(0, KCn, 16):
                skc = min(16, KCn - s0)
                slab, sk = self.slabs[self.slabn]
                self.slabn ^= 1
                src = Wd[krow0 + s0 * 128:krow0 + (s0 + skc) * 128, g0:g0 + gc].rearrange("(kc p) n -> p kc n", p=128)
                self.dma(slab[:, 0:skc, 0:gc], src, ["W" + wname], [sk])
                for ci in range(nch):
                    rows = min(128, gc - ci * 128)
                    pb, pk = banks[ci]

                    def mm(e, ci=ci, rows=rows, pb=pb, slab=slab, s0=s0, skc=skc):
                        m = None
                        for kc in range(skc):
                            m = e.matmul(pb[0:rows, 0:N], lhsT=slab[:, kc, ci * 128:ci * 128 + rows], rhs=rhs(s0 + kc),
                                         start=(s0 + kc == 0), stop=(s0 + kc == KCn - 1))
                        return m
                    P.add("pe", mm, reads=[sk] + list(rhs_keys), writes=[pk])
            if evac_group is not None:
                evac_group([((g0 + ci * 128) // 128, banks[ci][0][0:min(128, gc - ci * 128), 0:N], banks[ci][1]) for ci in range(nch)])
                continue
            for ci in range(nch):
                rows = min(128, gc - ci * 128)
                pb, pk = banks[ci]
                evac((g0 + ci * 128) // 128, pb[0:rows, 0:N], pk, rows)

    def gemm_tm(self, wname, K, c0, c1, act, act_keys, ntok, evac):
        P = self.P
        Wd = self.Wbf[wname]
        KCn = K // 128
        assert KCn <= 16
        for g0 in range(c0, c1, 512):
            gc = min(512, c1 - g0)
            slab, sk = self.slabs[self.slabn]
            self.slabn ^= 1
            src = Wd[:, g0:g0 + gc].rearrange("(kc p) n -> p kc n", p=128)
            self.dma(slab[:, 0:KCn, 0:gc], src, ["W" + wname], [sk])
            for t0 in range(0, ntok, 128):
                tn = min(128, ntok - t0)
                pb, pk = self.psum()

                def mm(e, pb=pb, slab=slab, t0=t0, tn=tn, gc=gc):
                    m = None
                    for kc in range(KCn):
                        m = e.matmul(pb[0:tn, 0:gc], lhsT=act[:, kc, t0:t0 + tn], rhs=slab[:, kc, 0:gc], start=(kc == 0), stop=(kc == KCn - 1))
                    return m
                P.add("pe", mm, reads=[sk] + list(act_keys), writes=[pk])
                evac(g0, t0, tn, pb[0:tn, 0:gc], pk, gc)

    def ssq(self, terms, N, keys):
        pb, pk = self.psum()

        def mm(e):
            m = None
            for i, (mi, ap) in enumerate(terms):
                m = e.matmul(pb[:, 0:N], lhsT=self.mats[:, mi, :], rhs=ap, start=(i == 0), stop=(i == len(terms) - 1))
            return m
        self.P.add("pe", mm, reads=list(keys) + ["mats"], writes=[pk])
        return pb, pk

    def rstd_from(self, pb, pk, N, epsn, out, ok):
        self.P.add("dve", lambda e: e.tensor_scalar(out=out, in0=pb[:, 0:N], scalar1=float(epsn), scalar2=None, op0=ALU.add), reads=[pk], writes=[ok])
        self.P.add("act", lambda e: e.activation(out=out, in_=out, func=AF.Ln), reads=[ok], writes=[ok])
        self.P.add("act", lambda e: e.activation(out=out, in_=out, func=AF.Exp, scale=-0.5), reads=[ok], writes=[ok])

    def stage_mod(self, layer):
        c = self.cfg
        P = self.P
        A = self.A
        A.reset()
        self.slabs = None
        KC, NBp = c.KC, c.NB + 1
        cb, kcb = A.alloc((KC, NBp), F32)
        ch, kch = A.alloc((KC, NBp), BF16)
        self.dma(cb, self.cvec.rearrange("(kc p) n -> p kc n", p=128), [], [kcb])
        P.add("act", lambda e: e.activation(out=ch, in_=cb, func=AF.Silu), reads=[kcb], writes=[kch])
        bname = "adab%d" % layer
        mod = self.mod

        def evac(ch_i, ps_ap, pk, rows):
            P.add("act", lambda e: e.activation(out=mod[:, ch_i, :], in_=ps_ap, func=AF.Identity, bias=self.vec(bname, ch_i), scale=1.0),
                  reads=[pk, "vecs"], writes=["mod"])
        self.gemm_fm("ada%d" % layer, c.D, 0, 6 * c.D, lambda kc: ch[:, kc, :], [kch], NBp, evac)
        sq = float(np.sqrt(c.D))
        for which, (comp, gname) in enumerate(((1, "mixg%d" % layer), (4, "ffng%d" % layer))):
            dst = self.moda[:, which * KC:(which + 1) * KC, :]
            src = mod[:, comp * KC:(comp + 1) * KC, :]
            P.add("dve", lambda e, dst=dst, src=src: e.tensor_scalar(out=dst, in0=src, scalar1=1.0, scalar2=sq, op0=ALU.add, op1=ALU.mult),
                  reads=["mod"], writes=["moda%d" % which])
            for j in range(NBp):
                P.add("dve", lambda e, dst=dst, j=j, gname=gname: e.tensor_tensor(out=dst[:, :, j], in0=dst[:, :, j], in1=self.vec(gname, 0, KC), op=ALU.mult),
                      reads=["moda%d" % which, "vecs"], writes=["moda%d" % which])
        P.flush()

    def norm_mod(self, xb, kx, N, which, mi, u, ku, sqb, ksq, rst, krst, tmps, router=None):
        c = self.cfg
        P = self.P
        KC = c.KC
        P.add("act", lambda e: e.activation(out=sqb[:, :, 0:N], in_=xb[:, :, 0:N], func=AF.Square), reads=[kx], writes=[ksq])
        pb, pk = self.ssq([(0, sqb[:, kc, 0:N]) for kc in range(KC)], N, [ksq])
        self.rstd_from(pb, pk, N, c.D * EPS, rst[:, 0:N], krst)
        shc = 0 if which == 0 else 3
        for kc in range(KC):
            tp, tk = tmps[kc % len(tmps)]
            P.add("dve", lambda e, kc=kc, tp=tp: e.tensor_tensor(out=tp[:, 0:N], in0=xb[:, kc, 0:N], in1=rst[:, 0:N], op=ALU.mult),
                  reads=[kx, krst], writes=[tk])
            if router is None:
                P.add("act", lambda e, kc=kc, tp=tp: e.activation(out=u[:, kc, 0:N], in_=tp[:, 0:N], func=AF.Identity,
                                                            scale=self.moda[:, which * KC + kc, mi:mi + 1], bias=self.mod[:, shc * KC + kc, mi:mi + 1]),
                      reads=[tk, "moda%d" % which, "mod"], writes=[ku + "_%d" % kc])
            else:
                pr, kpr = router
                P.add("act", lambda e, kc=kc, tp=tp: e.activation(out=tp[:, 0:N], in_=tp[:, 0:N], func=AF.Identity,
                                                            scale=self.moda[:, which * KC + kc, mi:mi + 1], bias=self.mod[:, shc * KC + kc, mi:mi + 1]),
                      reads=[tk, "moda%d" % which, "mod"], writes=[tk])
                P.add("pool", lambda e, kc=kc, tp=tp: e.tensor_copy(out=u[:, kc, 0:N], in_=tp[:, 0:N]), reads=[tk], writes=[ku + "_%d" % kc])
                rwv = self.vec("rw", kc * 8, 8)
                P.add("pe", lambda e, kc=kc, tp=tp, rwv=rwv: e.matmul(pr[0:8, 0:N], lhsT=rwv, rhs=tp[:, 0:N], start=(kc == 0), stop=(kc == KC - 1)),
                      reads=[tk, "vecs"], writes=[kpr])

    def blocks(self, tb=None):
        c = self.cfg
        tb = tb or c.TB
        out = []
        for b in range(c.NB):
            for t0 in range(0, c.S, tb):
                out.append((b, b * c.TOKB + t0, min(tb, c.S - t0), False, t0))
            out.append((b, b * c.TOKB + c.S, c.L, True, c.S))
        return out

    def load_x(self, layer, b, col0, N, is_ctx, pos0, xb, kx):
        c = self.cfg
        if layer == 0:
            src = self.cT_in[b] if is_ctx else self.xT_in[b][:, pos0:pos0 + N]
            if is_ctx:
                src = src[:, 0:N]
            rk = []
        else:
            src = self.xs[:, col0:col0 + N]
            rk = ["xs"]
        self.dma(xb[:, :, 0:N], src.rearrange("(kc p) n -> p kc n", p=128), rk, [kx])

    def rope_apply(self, src, ksrc, N, pidx, cosT, sinT, pos0, out, kout, tmp, ktmp):
        P = self.P
        pb, pk = self.psum()
        P.add("pe", lambda e: e.matmul(pb[:, 0:N], lhsT=self.matsf[:, pidx, :], rhs=src, start=True, stop=True), reads=[ksrc, "matsf"], writes=[pk])
        P.add("dve", lambda e: e.tensor_tensor(out=tmp[:, 0:N], in0=pb[:, 0:N], in1=sinT[:, pos0:pos0 + N], op=ALU.mult), reads=[pk, "rope"], writes=[ktmp])
        P.add("pool", lambda e: e.tensor_tensor(out=src, in0=src, in1=cosT[:, pos0:pos0 + N], op=ALU.mult), reads=[ksrc, "rope", pk], writes=[ksrc])
        P.add("dve", lambda e: e.tensor_tensor(out=out, in0=src, in1=tmp[:, 0:N], op=ALU.add), reads=[ksrc, ktmp], writes=[kout])

    def stage_L0A(self):
        c = self.cfg
        P, A = self.P, self.A
        A.reset()
        self.slabs = None
        self.cast_some(5)
        KC, TB = c.KC, min(256, c.TB)
        xb, kx = A.alloc((KC, TB), F32)
        sqb, ksq = A.alloc((KC, TB), BF16)
        u, ku = A.alloc((KC, TB), BF16)
        rst, krst = A.alloc((TB,), F32)
        tmps = [A.alloc((TB,), F32) for _ in range(3)]
        qa, kqa = A.alloc((6, TB), F32)
        qasq, kqasq = A.alloc((6, TB), BF16)
        qn, kqn = A.alloc((6, TB), BF16)
        cva, kcva = A.alloc((8, TB), F32)
        sg, ksg = A.alloc((TB,), F32)
        hb, khb = A.alloc((2, TB), F32)
        kpe, kkpe = A.alloc((TB,), F32)
        qr, kqr = A.alloc((12, TB), F32)
        qsq, kqsq = A.alloc((12, TB), BF16)
        qo, kqo = A.alloc((12, TB), BF16)
        vb, kvb = A.alloc((max(2, (max(TB, c.L) + 127) // 128), 1024), BF16)
        self.hts = [A.alloc((TB,), F32) for _ in range(12)]
        rope, _ = A.alloc((2, c.TOKB), F32)
        self.dma(rope, self.rope_in[:, 0:2, :], [], ["rope"])
        cosT, sinT = rope[:, 0, :], rope[:, 1, :]
        g = self.gsc
        sc = float(192 ** -0.5)
        for (dst, src, n, f) in ((0, "qag", 4, 512 ** 0.5), (4, "kvag", 2, 256 ** 0.5), (6, "qng_n", 1, 192 ** 0.5 * sc), (7, "qng_r", 1, 192 ** 0.5 * sc),
                                 (8, "kng_n", 1, 192 ** 0.5), (9, "kng_r", 1, 192 ** 0.5)):
            P.add("dve", lambda e, dst=dst, src=src, n=n, f=f: e.tensor_scalar(out=g[:, dst:dst + n], in0=self.vec(src, 0, n), scalar1=float(f), scalar2=None, op0=ALU.mult),
                  reads=["vecs"], writes=["gsc"])
        for (b, col0, N, is_ctx, pos0) in self.blocks(TB):
            mi = c.NB if is_ctx else b
            self.load_x(0, b, col0, N, is_ctx, pos0, xb, kx)
            self.norm_mod(xb, kx, N, 0, mi, u, ku, sqb, ksq, rst, krst, tmps)
            ukeys = [ku + "_%d" % kc for kc in range(KC)]

            def evac(ch, ps_ap, pk, rows, N=N, col0=col0):
                if ch < 6:
                    P.add("act", lambda e: e.activation(out=qa[:, ch, 0:N], in_=ps_ap, func=AF.Copy), reads=[pk], writes=[kqa + str(ch)])
                elif ch < 14:
                    P.add("act", lambda e: e.activation(out=cva[:, ch - 6, 0:N], in_=ps_ap, func=AF.Copy), reads=[pk], writes=[kcva + str(ch)])
                elif ch < 22:
                    j = ch - 14
                    hbj = hb[:, j % 2, 0:N]
                    P.add("act", lambda e: e.activation(out=sg[:, 0:N], in_=ps_ap, func=AF.Sigmoid), reads=[pk], writes=[ksg])
                    P.add("dve", lambda e: e.tensor_tensor(out=hbj, in0=sg[:, 0:N], in1=cva[:, j, 0:N], op=ALU.mult), reads=[ksg, kcva + str(j + 6)], writes=[khb + str(j % 2)])
                    self.dma(self.hT[j * 128:(j + 1) * 128, col0:col0 + N], hbj, [khb + str(j % 2)], ["hT"])
                else:
                    P.add("act", lambda e: e.activation(out=kpe[:, 0:N], in_=ps_ap, func=AF.Copy), reads=[pk], writes=[kkpe])
            self.gemm_fm("win0", c.D, 0, 2944, lambda kc, N=N: u[:, kc, 0:N], ukeys, N, evac)
            P.add("act", lambda e, N=N: e.activation(out=qasq[:, :, 0:N], in_=qa[:, :, 0:N], func=AF.Square), reads=[kqa + str(i) for i in range(6)], writes=[kqasq])
            for (lo, hi, epsn) in ((0, 4, 512 * EPS), (4, 6, 256 * EPS)):
                pb, pk = self.ssq([(0, qasq[:, kc, 0:N]) for kc in range(lo, hi)], N, [kqasq])
                self.rstd_from(pb, pk, N, epsn, rst[:, 0:N], krst)
                for kc in range(lo, hi):
                    P.add("dve", lambda e, kc=kc, N=N: e.scalar_tensor_tensor(out=qn[:, kc, 0:N], in0=qa[:, kc, 0:N], scalar=g[:, kc:kc + 1], in1=rst[:, 0:N], op0=ALU.mult, op1=ALU.mult),
                          reads=[kqa + str(kc), krst, "gsc"], writes=[kqn + str(kc)])
            def evq(ch, ps_ap, pk, rows, N=N):
                P.add("act", lambda e: e.activation(out=qr[:, ch, 0:N], in_=ps_ap, func=AF.Copy), reads=[pk], writes=[kqr + str(ch)])
            self.gemm_fm("wqb", 512, 0, 1536, lambda kc, N=N: qn[:, kc, 0:N], [kqn + str(i) for i in range(4)], N, evq)
            self.head_norm_rope(qr, kqr, qsq, kqsq, qo, kqo, N, 6, 7, cosT, sinT, pos0, rst, krst, tmps, None, None)
            self.dma(self.qT[:, col0:col0 + N].rearrange("(ch p) n -> p ch n", p=128), qo[:, :, 0:N], [kqo + str(i) for i in range(12)], ["qT"])
            def evk(ch, ps_ap, pk, rows, N=N):
                P.add("act", lambda e: e.activation(out=qr[:, ch, 0:N], in_=ps_ap, func=AF.Copy), reads=[pk], writes=[kqr + str(ch)])
            self.gemm_fm("wkvb", 256, 0, 1024, lambda kc, N=N: qn[:, 4 + kc, 0:N], [kqn + "4", kqn + "5"], N, evk)
            self.head_norm_rope(qr, kqr, qsq, kqsq, qo, kqo, N, 8, 9, cosT, sinT, pos0, rst, krst, tmps, kpe, kkpe)
            self.dma(self.kT[:, col0:col0 + N].rearrange("(ch p) n -> p ch n", p=128), qo[:, :, 0:N], [kqo + str(i) for i in range(12)], ["kT"])

            def evv(g0, t0, tn, ps_ap, pk, gc, col0=col0):
                ti = t0 // 128
                P.add("act", lambda e: e.activation(out=vb[0:tn, ti, g0 - 1024:g0 - 1024 + gc], in_=ps_ap, func=AF.Copy), reads=[pk], writes=[kvb + "%d_%d" % (ti, g0)])
                if g0 + gc >= 2048:
                    self.dma(self.Vt[col0 + t0:col0 + t0 + tn, :], vb[0:tn, ti, :], [kvb + "%d_%d" % (ti, gg) for gg in (1024, 1536)], ["Vt"])
            self.gemm_tm("wkvb", 256, 1024, 2048, qn[:, 4:6, :], [kqn + "4", kqn + "5"], N, evv)
        P.flush()

    def head_norm_rope(self, qr, kqr, qsq, kqsq, qo, kqo, N, gn, gr, cosT, sinT, pos0, rst, krst, tmps, kpe, kkpe):
        P = self.P
        g = self.gsc
        is_k = kpe is not None
        nsq = 8 if is_k else 12
        P.add("act", lambda e: e.activation(out=qsq[:, 0:nsq, 0:N], in_=qr[:, 0:nsq, 0:N], func=AF.Square), reads=[kqr + str(i) for i in range(nsq)], writes=[kqsq])
        if is_k:
            P.add("act", lambda e: e.activation(out=qsq[:, 8, 0:N], in_=kpe[:, 0:N], func=AF.Square), reads=[kkpe], writes=[kqsq + "p"])
            P.add("dve", lambda e: e.tensor_scalar(out=qr[:, 8, 0:N], in0=kpe[:, 0:N], scalar1=g[:, gr:gr + 1], scalar2=None, op0=ALU.mult), reads=[kkpe, "gsc"], writes=[kqr + "8"])
            tp, tk = tmps[0]
            pb, pk = self.psum()
            src = qr[:, 8, 0:N]
            P.add("pe", lambda e: e.matmul(pb[:, 0:N], lhsT=self.matsf[:, 0, :], rhs=src, start=True, stop=True), reads=[kqr + "8", "matsf"], writes=[pk])
            P.add("dve", lambda e: e.tensor_tensor(out=tp[:, 0:N], in0=pb[:, 0:N], in1=sinT[:, pos0:pos0 + N], op=ALU.mult), reads=[pk, "rope"], writes=[tk])
            P.add("dve", lambda e: e.tensor_tensor(out=src, in0=src, in1=cosT[:, pos0:pos0 + N], op=ALU.mult), reads=[kqr + "8", "rope", pk], writes=[kqr + "8"])
            P.add("dve", lambda e: e.tensor_tensor(out=src, in0=src, in1=tp[:, 0:N], op=ALU.add), reads=[kqr + "8", tk], writes=[kqr + "8"])
        hts = self.hts
        hp_ = []
        for h in range(8):
            rsq = qsq[:, 8, 0:N] if is_k else qsq[:, 8 + h // 2, 0:N]
            rkey = (kqsq + "p") if is_k else kqsq
            topbot = 1 if (is_k or h % 2 == 0) else 2
            hp_.append(self.ssq([(0, qsq[:, h, 0:N]), (topbot, rsq)], N, [kqsq, rkey]))
        for h in range(8):
            tp, tk = hts[h]
            P.add("dve", lambda e, h=h, tp=tp: e.tensor_scalar(out=tp[:, 0:N], in0=hp_[h][0][:, 0:N], scalar1=float(192 * EPS), scalar2=None, op0=ALU.add), reads=[hp_[h][1]], writes=[tk])
        pp_ = []
        for j in range(4):
            rsq = qsq[:, 8, 0:N] if is_k else qsq[:, 8 + j, 0:N]
            pp_.append(self.ssq([(3, qsq[:, 2 * j, 0:N]), (4, qsq[:, 2 * j + 1, 0:N]), (5, rsq)], N, [kqsq, kqsq + "p"] if is_k else [kqsq]))
        for j in range(4):
            tp, tk = hts[8 + j]
            P.add("dve", lambda e, j=j, tp=tp: e.tensor_scalar(out=tp[:, 0:N], in0=pp_[j][0][:, 0:N], scalar1=float(192 * EPS), scalar2=None, op0=ALU.add), reads=[pp_[j][1]], writes=[tk])
        for i in range(12):
            tp, tk = hts[i]
            P.add("act", lambda e, tp=tp: e.activation(out=tp[:, 0:N], in_=tp[:, 0:N], func=AF.Ln), reads=[tk], writes=[tk])
        for i in range(12):
            tp, tk = hts[i]
            P.add("act", lambda e, tp=tp: e.activation(out=tp[:, 0:N], in_=tp[:, 0:N], func=AF.Exp, scale=-0.5), reads=[tk], writes=[tk])
        for h in range(8):
            tp, tk = hts[h]
            P.add("dve", lambda e, h=h, tp=tp: e.scalar_tensor_tensor(out=qo[:, h, 0:N], in0=qr[:, h, 0:N], scalar=g[:, gn:gn + 1], in1=tp[:, 0:N], op0=ALU.mult, op1=ALU.mult),
                  reads=[kqr + str(h), tk, "gsc"], writes=[kqo + str(h)])
        if is_k:
            for j in range(4):
                tp, tk = hts[8 + j]
                P.add("dve", lambda e, j=j, tp=tp: e.tensor_tensor(out=qo[:, 8 + j, 0:N], in0=qr[:, 8, 0:N], in1=tp[:, 0:N], op=ALU.mult),
                      reads=[kqr + "8", tk], writes=[kqo + str(8 + j)])
        else:
            for j in range(4):
                tp, tk = hts[8 + j]
                src = qr[:, 8 + j, 0:N]
                P.add("dve", lambda e, src=src, tp=tp: e.scalar_tensor_tensor(out=src, in0=src, scalar=g[:, gr:gr + 1], in1=tp[:, 0:N], op0=ALU.mult, op1=ALU.mult),
                      reads=[kqr + str(8 + j), tk, "gsc"], writes=[kqr + str(8 + j)])
            pr_ = []
            for j in range(4):
                src = qr[:, 8 + j, 0:N]
                pb2, pk2 = self.psum()
                pr_.append((pb2, pk2))
                P.add("pe", lambda e, src=src, pb2=pb2: e.matmul(pb2[:, 0:N], lhsT=self.matsf[:, 0, :], rhs=src, start=True, stop=True), reads=[kqr + str(8 + j), "matsf"], writes=[pk2])
            for j in range(4):
                tp, tk = hts[8 + j]
                src = qr[:, 8 + j, 0:N]
                pb2, pk2 = pr_[j]
                P.add("dve", lambda e, pb2=pb2, tp=tp: e.tensor_tensor(out=tp[:, 0:N], in0=pb2[:, 0:N], in1=sinT[:, pos0:pos0 + N], op=ALU.mult), reads=[pk2, "rope", tk], writes=[tk])
                P.add("dve", lambda e, src=src: e.tensor_tensor(out=src, in0=src, in1=cosT[:, pos0:pos0 + N], op=ALU.mult), reads=[kqr + str(8 + j), "rope", pk2], writes=[kqr + str(8 + j)])
            for j in range(4):
                tp, tk = hts[8 + j]
                src = qr[:, 8 + j, 0:N]
                P.add("dve", lambda e, src=src, tp=tp, j=j: e.tensor_tensor(out=qo[:, 8 + j, 0:N], in0=src, in1=tp[:, 0:N], op=ALU.add), reads=[kqr + str(8 + j), tk], writes=[kqo + str(8 + j)])

    def stage_L0B(self, merged=False):
        c = self.cfg
        P, A = self.P, self.A
        if not merged:
            A.reset()
            self.slabs = None
        self.cast_some(4)
        NKC = c.TOKB // 128
        TB = c.TB
        kn, kkn = A.alloc((c.TOKB,), BF16)
        kr, kkr = A.alloc((c.TOKB,), BF16)
        vh, kvh = A.alloc((NKC, 128), BF16)
        qn, kqn = A.alloc((TB,), BF16)
        qr, kqr = A.alloc((TB,), BF16)
        pts = [A.alloc((TB,), BF16) for _ in range(4)]
        rden, krden = A.alloc((TB,), F32)
        ob, kob = A.alloc((TB,), BF16)
        self.ps_lo = 2
        self.psn = 2
        pO, kO, pD, kD = self.ps[0], "ps0", self.ps[1], "ps1"
        for b in range(c.NB):
            cb = b * c.TOKB
            for h in range(8):
                yield
                hp = (h % 2) * 64
                self.dma(kn, self.kT[h * 128:(h + 1) * 128, cb:cb + c.TOKB], ["kT"], [kkn])
                self.dma(kr, self.kT[1024 + (h // 2) * 128:1024 + (h // 2) * 128 + 128, cb:cb + c.TOKB], ["kT"], [kkr])
                self.dma(vh, self.Vt[cb:cb + c.TOKB, h * 128:(h + 1) * 128].rearrange("(kc p) d -> p kc d", p=128), ["Vt"], [kvh])
                for (bb, col0, N, is_ctx, pos0) in self.blocks():
                    if bb != b:
                        continue
                    self.dma(qn[:, 0:N], self.qT[h * 128:(h + 1) * 128, col0:col0 + N], ["qT"], [kqn])
                    self.dma(qr[:, 0:N], self.qT[1024 + (h // 2) * 128:1024 + (h // 2) * 128 + 128, col0:col0 + N], ["qT"], [kqr])
                    kcs = list(range(c.S // 128, NKC)) if is_ctx else list(range(NKC))
                    pend = []
                    for i, kc in enumerate(kcs):
                        pS, kS = self.psum()
                        pt, kpt = pts[i % 4]

                        def mmS(e, pS=pS, kc=kc, N=N):
                            e.matmul(pS[:, 0:N], lhsT=kn[:, kc * 128:(kc + 1) * 128], rhs=qn[:, 0:N], start=True, stop=False)
                            return e.matmul(pS[:, 0:N], lhsT=kr[hp:hp + 64, kc * 128:(kc + 1) * 128], rhs=qr[hp:hp + 64, 0:N], start=False, stop=True)
                        P.add("pe", mmS, reads=[kkn, kkr, kqn, kqr], writes=[kS])
                        P.add("act", lambda e, pS=pS, pt=pt, N=N: e.activation(out=pt[:, 0:N], in_=pS[:, 0:N], func=AF.Exp), reads=[kS], writes=[kpt])

                        def mmO(e, pt=pt, kc=kc, N=N, first=(i == 0), last=(i == len(kcs) - 1)):
                            e.matmul(pO[:, 0:N], lhsT=vh[:, kc, :], rhs=pt[:, 0:N], start=first, stop=last)
                            return e.matmul(pD[:, 0:N], lhsT=self.mats[:, 0, :], rhs=pt[:, 0:N], start=first, stop=last)
                        pend.append((mmO, kpt))
                        if len(pend) > 2:
                            fn, kp = pend.pop(0)
                            P.add("pe", fn, reads=[kvh, kp, "mats"], writes=[kO, kD])
                    for fn, kp in pend:
                        P.add("pe", fn, reads=[kvh, kp, "mats"], writes=[kO, kD])
                    P.add("dve", lambda e, N=N: e.reciprocal(out=rden[:, 0:N], in_=pD[:, 0:N]), reads=[kD], writes=[krden])
                    P.add("dve", lambda e, N=N: e.tensor_tensor(out=ob[:, 0:N], in0=pO[:, 0:N], in1=rden[:, 0:N], op=ALU.mult), reads=[kO, krden], writes=[kob])
                    self.dma(self.mixT[h * 128:(h + 1) * 128, col0:col0 + N], ob[:, 0:N], [kob], ["mixT"])
        if not merged:
            self.ps_lo = 0
            P.flush()
        yield

    def stage_L0C(self, merged=False):
        c = self.cfg
        P, A = self.P, self.A
        if not merged:
            A.reset()
            self.slabs = None
        S = c.S
        hp, khp = A.alloc((S + 30,), F32)
        acc2, kacc2 = A.alloc((S,), F32)
        cvb, kcvb = A.alloc((8, S), F32)
        sqt, ksqt = A.alloc((8, 512), F32)
        mean, kmean = A.alloc((512,), F32)
        rst, krst = A.alloc((512,), F32)
        t1, kt1 = A.alloc((512,), F32)
        yo, kyo = A.alloc((8, 512), BF16)
        for b in range(c.NB):
            for (pos0, ln) in ((0, S), (S, c.L)):
                col = b * c.TOKB + pos0
                for j in range(8):
                    yield
                    P.add("pool", lambda e: e.memset(hp[:, 0:15], 0.0), reads=[], writes=[khp])
                    P.add("pool", lambda e, ln=ln: e.memset(hp[:, 15 + ln:30 + ln], 0.0), reads=[], writes=[khp])
                    self.dma(hp[:, 15:15 + ln], self.hT[j * 128:(j + 1) * 128, col:col + ln], ["hT"], [khp])
                    acc = cvb[:, j, 0:ln]
                    w = lambda t, j=j: self.vec("dww", j * 31 + t)
                    P.add("dve", lambda e, acc=acc, ln=ln, j=j, w=w: e.tensor_scalar(out=acc, in0=hp[:, 0:ln], scalar1=w(0), scalar2=self.vec("dwb", j), op0=ALU.mult, op1=ALU.add),
                          reads=[khp, "vecs"], writes=[kcvb + str(j)])
                    for t in range(1, 31):
                        P.add("dve", lambda e, acc=acc, ln=ln, t=t, w=w: e.scalar_tensor_tensor(out=acc, in0=hp[:, t:t + ln], scalar=w(t), in1=acc, op0=ALU.mult, op1=ALU.add),
                              reads=[khp, "vecs", kcvb + str(j)], writes=[kcvb + str(j)])
                for t0 in range(0, ln, 512):
                    yield
                    N = min(512, ln - t0)
                    ck = [kcvb + str(j) for j in range(8)]
                    P.add("act", lambda e, t0=t0, N=N: e.activation(out=sqt[:, :, 0:N], in_=cvb[:, :, t0:t0 + N], func=AF.Square), reads=ck, writes=[ksqt + str(j) for j in range(8)])
                    p1, k1 = self.psum()
                    p2, k2 = self.psum()

                    def mm1(e, p1=p1, t0=t0, N=N):
                        m = None
                        for j in range(8):
                            m = e.matmul(p1[:, 0:N], lhsT=self.matsf[:, 3, :], rhs=cvb[:, j, t0:t0 + N], start=(j == 0), stop=(j == 7))
                        return m

                    def mm2(e, p2=p2, N=N):
                        m = None
                        for j in range(8):
                            m = e.matmul(p2[:, 0:N], lhsT=self.matsf[:, 3, :], rhs=sqt[:, j, 0:N], start=(j == 0), stop=(j == 7))
                        return m
                    P.add("pe", mm1, reads=ck + ["matsf"], writes=[k1])
                    P.add("pe", mm2, reads=[ksqt + str(j) for j in range(8)] + ["matsf"], writes=[k2])
                    P.add("dve", lambda e, p1=p1, N=N: e.tensor_scalar(out=mean[:, 0:N], in0=p1[:, 0:N], scalar1=1.0 / 1024, scalar2=None, op0=ALU.mult), reads=[k1], writes=[kmean])
                    P.add("dve", lambda e, N=N: e.tensor_tensor(out=t1[:, 0:N], in0=mean[:, 0:N], in1=mean[:, 0:N], op=ALU.mult), reads=[kmean], writes=[kt1])
                    P.add("dve", lambda e, p2=p2, N=N: e.scalar_tensor_tensor(out=rst[:, 0:N], in0=p2[:, 0:N], scalar=1.0 / 1024, in1=t1[:, 0:N], op0=ALU.mult, op1=ALU.subtract),
                          reads=[k2, kt1], writes=[krst])
                    P.add("dve", lambda e, N=N: e.tensor_scalar(out=rst[:, 0:N], in0=rst[:, 0:N], scalar1=EPS, scalar2=None, op0=ALU.add), reads=[krst], writes=[krst])
                    P.add("act", lambda e, N=N: e.activation(out=rst[:, 0:N], in_=rst[:, 0:N], func=AF.Ln), reads=[krst], writes=[krst])
                    P.add("act", lambda e, N=N: e.activation(out=rst[:, 0:N], in_=rst[:, 0:N], func=AF.Exp, scale=-0.5), reads=[krst], writes=[krst])
                    for j in range(8):
                        P.add("dve", lambda e, j=j, N=N, t0=t0: e.tensor_tensor(out=sqt[:, j, 0:N], in0=cvb[:, j, t0:t0 + N], in1=mean[:, 0:N], op=ALU.subtract),
                              reads=[kcvb + str(j), kmean], writes=[ksqt + str(j)])
                        P.add("dve", lambda e, j=j, N=N: e.tensor_tensor(out=sqt[:, j, 0:N], in0=sqt[:, j, 0:N], in1=rst[:, 0:N], op=ALU.mult),
                              reads=[ksqt + str(j), krst], writes=[ksqt + str(j)])
                        P.add("act", lambda e, j=j, N=N: e.activation(out=yo[:, j, 0:N], in_=sqt[:, j, 0:N], func=AF.Silu, scale=self.vec("lng", j), bias=self.vec("lnb", j)),
                              reads=[ksqt + str(j), "vecs"], writes=[kyo + str(j)])
                    self.dma(self.mixT[1024:2048, col + t0:col + t0 + N].rearrange("(ch p) n -> p ch n", p=128), yo[:, :, 0:N], [kyo + str(j) for j in range(8)], ["mixT"])
        if not merged:
            P.flush()
        yield

    def stage_D(self, layer):
        c = self.cfg
        P, A = self.P, self.A
        A.reset()
        self.slabs = None
        self.cast_some(5 if layer == 0 else 99)
        KC, TB = c.KC, c.TB
        FC = c.DFF // 128 if layer == 0 else c.EFF // 128 // 4
        xb, kx = A.alloc((KC, TB), F32)
        mx, kmx = A.alloc((16, TB), BF16)
        sqb, ksq = A.alloc((KC, TB), BF16)
        f, kf = A.alloc((KC, TB), BF16)
        rst, krst = A.alloc((TB,), F32)
        tmps = [A.alloc((TB,), F32) for _ in range(3 if layer == 0 else 2)]
        hid, khid = A.alloc((FC, TB), BF16)
        wout = "wout%d" % layer
        if layer == 1:
            NT = TB // 128
            acc, kacc = A.alloc((KC, TB), F32)
            lg, klg = A.alloc((TB,), F32)
            lt, klt = A.alloc((NT, 8), F32)
            mx8, kmx8 = A.alloc((NT, 8), F32)
            ng, kng = A.alloc((NT,), F32)
            msk, kmsk = A.alloc((NT, 8), F32)
            den, kden = A.alloc((NT,), F32)
            GT, kGT = A.alloc((TB,), F32)
            gbc, kgbc = A.alloc((TB,), F32)
            selm, _ = A.alloc((8, 128), F32)
            self.dma(selm[0:8], self.selm_in, [], ["selm"])
            self.ps_lo = 1
            self.psn = 1
            pr, kpr = self.ps[0], "ps0"
        for (b, col0, N, is_ctx, pos0) in self.blocks():
            if layer == 1 and is_ctx:
                continue
            mi = c.NB if is_ctx else b
            self.load_x(layer, b, col0, N, is_ctx, pos0, xb, kx)
            self.dma(mx[:, :, 0:N], self.mixT[:, col0:col0 + N].rearrange("(ch p) n -> p ch n", p=128), ["mixT"], [kmx])

            def ev1(ch, ps_ap, pk, rows, N=N, mi=mi):
                P.add("dve", lambda e: e.scalar_tensor_tensor(out=xb[:, ch, 0:N], in0=ps_ap, scalar=self.mod[:, 2 * KC + ch, mi:mi + 1], in1=xb[:, ch, 0:N], op0=ALU.mult, op1=ALU.add),
                      reads=[pk, kx, "mod"], writes=[kx])
            self.gemm_fm(wout, 2048, 0, c.D, lambda kc, N=N: mx[:, kc, 0:N], [kmx], N, ev1)
            self.norm_mod(xb, kx, N, 1, mi, f, kf, sqb, ksq, rst, krst, tmps, router=((pr, kpr) if layer == 1 else None))
            fkeys = [kf + "_%d" % kc for kc in range(KC)]
            if layer == 1:
                self.moe_block(b, col0, N, pos0, mi, xb, kx, f, fkeys, hid, khid, FC, acc, kacc, lg, klg, lt, klt, mx8, kmx8, ng, kng, msk, kmsk, den, kden, GT, kGT, gbc, kgbc, selm, pr, kpr)
            if layer == 0:
                def evg(ch, ps_ap, pk, rows, N=N):
                    P.add("act", lambda e: e.activation(out=hid[:, ch, 0:N], in_=ps_ap, func=AF.Silu), reads=[pk], writes=[khid + str(ch)])

                def evu(ch, ps_ap, pk, rows, N=N):
                    P.add("dve", lambda e: e.tensor_tensor(out=hid[:, ch, 0:N], in0=ps_ap, in1=hid[:, ch, 0:N], op=ALU.mult), reads=[pk, khid + str(ch)], writes=[khid + str(ch)])
                self.gemm_fm("dg", c.D, 0, c.DFF, lambda kc, N=N: f[:, kc, 0:N], fkeys, N, evg)
                self.gemm_fm("du", c.D, 0, c.DFF, lambda kc, N=N: f[:, kc, 0:N], fkeys, N, evu)

                def evd(ch, ps_ap, pk, rows, N=N, mi=mi):
                    P.add("dve", lambda e: e.scalar_tensor_tensor(out=xb[:, ch, 0:N], in0=ps_ap, scalar=self.mod[:, 5 * KC + ch, mi:mi + 1], in1=xb[:, ch, 0:N], op0=ALU.mult, op1=ALU.add),
                          reads=[pk, kx, "mod"], writes=[kx])
                self.gemm_fm("dd", c.DFF, 0, c.D, lambda kc, N=N: hid[:, kc, 0:N], [khid + str(i) for i in range(FC)], N, evd)
                self.dma(self.xs[:, col0:col0 + N].rearrange("(kc p) n -> p kc n", p=128), xb[:, :, 0:N], [kx], ["xs"])
        self.ps_lo = 0
        P.flush()

    def moe_block(self, b, col0, N, pos0, mi, xb, kx, f, fkeys, hid, khid, FC, acc, kacc, lg, klg, lt, klt, mx8, kmx8, ng, kng, msk, kmsk, den, kden, GT, kGT, gbc, kgbc, selm, pr, kpr):
        c = self.cfg
        P = self.P
        KC = c.KC
        NT = (N + 127) // 128
        X = mybir.AxisListType.X
        P.add("act", lambda e: e.activation(out=lg[0:8, 0:N], in_=pr[0:8, 0:N], func=AF.Copy), reads=[kpr], writes=[klg])
        for tt in range(NT):
            tn = min(128, N - tt * 128)
            pT, kT_ = self.psum()
            P.add("pe", lambda e, pT=pT, tt=tt, tn=tn: e.transpose(pT[0:tn, 0:8], lg[0:8, tt * 128:tt * 128 + tn], self.matsf[0:8, 2, 0:8]), reads=[klg, "matsf"], writes=[kT_])
            P.add("act", lambda e, pT=pT, tt=tt, tn=tn: e.activation(out=lt[0:tn, tt, :], in_=pT[0:tn, 0:8], func=AF.Copy), reads=[kT_], writes=[klt])
            P.add("dve", lambda e, tt=tt, tn=tn: e.max(out=mx8[0:tn, tt, :], in_=lt[0:tn, tt, :]), reads=[klt], writes=[kmx8])
            P.add("dve", lambda e, tt=tt, tn=tn: e.tensor_scalar(out=ng[0:tn, tt:tt + 1], in0=mx8[0:tn, tt, 0:1], scalar1=-1.0, scalar2=None, op0=ALU.mult), reads=[kmx8], writes=[kng])
            P.add("dve", lambda e, tt=tt, tn=tn: e.tensor_scalar(out=msk[0:tn, tt, :], in0=lt[0:tn, tt, :], scalar1=mx8[0:tn, tt, 1:2], scalar2=None, op0=ALU.is_ge), reads=[klt, kmx8], writes=[kmsk])
            P.add("act", lambda e, tt=tt, tn=tn: e.activation(out=lt[0:tn, tt, :], in_=lt[0:tn, tt, :], func=AF.Exp, bias=ng[0:tn, tt:tt + 1], scale=1.0), reads=[klt, kng, kmsk], writes=[klt])
            P.add("dve", lambda e, tt=tt, tn=tn: e.tensor_tensor(out=msk[0:tn, tt, :], in0=msk[0:tn, tt, :], in1=lt[0:tn, tt, :], op=ALU.mult), reads=[klt, kmsk], writes=[kmsk])
            P.add("dve", lambda e, tt=tt, tn=tn: e.reduce_sum(out=den[0:tn, tt:tt + 1], in_=msk[0:tn, tt, :], axis=X), reads=[kmsk], writes=[kden])
            P.add("dve", lambda e, tt=tt, tn=tn: e.reciprocal(out=den[0:tn, tt:tt + 1], in_=den[0:tn, tt:tt + 1]), reads=[kden], writes=[kden])
            P.add("dve", lambda e, tt=tt, tn=tn: e.tensor_scalar(out=msk[0:tn, tt, :], in0=msk[0:tn, tt, :], scalar1=den[0:tn, tt:tt + 1], scalar2=None, op0=ALU.mult), reads=[kmsk, kden], writes=[kmsk])
            pG, kG = self.psum()
            P.add("pe", lambda e, pG=pG, tt=tt, tn=tn: e.transpose(pG[0:8, 0:tn], msk[0:tn, tt, :], self.matsf[0:tn, 2, 0:tn]), reads=[kmsk, "matsf"], writes=[kG])
            P.add("act", lambda e, pG=pG, tt=tt, tn=tn: e.activation(out=GT[0:8, tt * 128:tt * 128 + tn], in_=pG[0:8, 0:tn], func=AF.Copy), reads=[kG], writes=[kGT])
        NH = 4
        FCq = FC
        first = True
        for ex in range(c.E):
            pg, kg = self.psum()
            P.add("pe", lambda e, pg=pg, ex=ex: e.matmul(pg[:, 0:N], lhsT=selm[0:8, ex, :], rhs=GT[0:8, 0:N], start=True, stop=True), reads=[kGT, "selm"], writes=[kg])
            P.add("act", lambda e, pg=pg: e.activation(out=gbc[:, 0:N], in_=pg[:, 0:N], func=AF.Copy), reads=[kg], writes=[kgbc])
            for hq in range(NH):
                f0 = hq * FCq * 128

                def evg(ch, ps_ap, pk, rows, f0=f0):
                    cl = ch - f0 // 128
                    P.add("act", lambda e: e.activation(out=hid[:, cl, 0:N], in_=ps_ap, func=AF.Silu), reads=[pk], writes=[khid + str(cl)])

                def evu(ch, ps_ap, pk, rows, f0=f0):
                    cl = ch - f0 // 128
                    P.add("dve", lambda e: e.tensor_tensor(out=hid[:, cl, 0:N], in0=ps_ap, in1=hid[:, cl, 0:N], op=ALU.mult), reads=[pk, khid + str(cl)], writes=[khid + str(cl)])
                self.gemm_fm("eg%d" % ex, c.D, f0, f0 + FCq * 128, lambda kc: f[:, kc, 0:N], fkeys, N, evg)
                self.gemm_fm("eu%d" % ex, c.D, f0, f0 + FCq * 128, lambda kc: f[:, kc, 0:N], fkeys, N, evu)

                def evd(ch, ps_ap, pk, rows, first=first):
                    if first:
                        P.add("dve", lambda e: e.tensor_tensor(out=acc[:, ch, 0:N], in0=ps_ap, in1=gbc[:, 0:N], op=ALU.mult), reads=[pk, kgbc], writes=[kacc + str(ch)])
                    else:
                        P.add("dve", lambda e: e.tensor_tensor(out=lg[:, 0:N], in0=ps_ap, in1=gbc[:, 0:N], op=ALU.mult), reads=[pk, kgbc, klg], writes=[klg])
                        P.add("pool", lambda e: e.tensor_tensor(out=acc[:, ch, 0:N], in0=acc[:, ch, 0:N], in1=lg[:, 0:N], op=ALU.add), reads=[klg, kacc + str(ch)], writes=[kacc + str(ch)])
                self.gemm_fm("ed%d" % ex, FCq * 128, 0, c.D, lambda kc: hid[:, kc, 0:N], [khid + str(i) for i in range(FCq)], N, evd, krow0=f0)
                first = False
        for ch in range(KC):
            P.add("dve", lambda e, ch=ch: e.scalar_tensor_tensor(out=xb[:, ch, 0:N], in0=acc[:, ch, 0:N], scalar=self.mod[:, 5 * KC + ch, mi:mi + 1], in1=xb[:, ch, 0:N], op0=ALU.mult, op1=ALU.add),
                  reads=[kacc + str(ch), kx, "mod"], writes=[kx])
        self.dma(self.outT[b][:, pos0:pos0 + N].rearrange("(kc p) n -> p kc n", p=128), xb[:, :, 0:N], [kx], ["outT"])

    def stage_L1A(self):
        c = self.cfg
        P, A = self.P, self.A
        A.reset()
        self.slabs = None
        self.cast_some(4)
        KC, TB = c.KC, c.TB
        xb, kx = A.alloc((KC, TB), F32)
        sqb, ksq = A.alloc((KC, TB), BF16)
        u, ku = A.alloc((KC, TB), BF16)
        rst, krst = A.alloc((TB,), F32)
        tmps = [A.alloc((TB,), F32) for _ in range(3)]
        qr, kqr = A.alloc((4, TB), F32)
        qsq, kqsq = A.alloc((4, TB), BF16)
        qo, kqo = A.alloc((4, TB), BF16)
        rts = [A.alloc((TB,), F32) for _ in range(4)]
        vb, kvb = A.alloc((4, 512), BF16)
        rope, _ = A.alloc((2, c.TOKB), F32)
        self.dma(rope, self.rope_in[:, 2:4, :], [], ["rope"])
        cosT, sinT = rope[:, 0, :], rope[:, 1, :]
        g = self.gsc
        P.add("dve", lambda e: e.tensor_scalar(out=g[:, 10:11], in0=self.vec("sqg"), scalar1=1.0, scalar2=None, op0=ALU.mult), reads=["vecs"], writes=["gsc"])
        P.add("dve", lambda e: e.tensor_scalar(out=g[:, 11:12], in0=self.vec("skg"), scalar1=float(128 ** 0.5), scalar2=None, op0=ALU.mult), reads=["vecs"], writes=["gsc"])
        for (b, col0, N, is_ctx, pos0) in self.blocks():
            mi = c.NB if is_ctx else b
            self.load_x(1, b, col0, N, is_ctx, pos0, xb, kx)
            self.norm_mod(xb, kx, N, 0, mi, u, ku, sqb, ksq, rst, krst, tmps)
            ukeys = [ku + "_%d" % kc for kc in range(KC)]

            def evac(ch, ps_ap, pk, rows, N=N, col0=col0, pos0=pos0):
                if ch < 16:
                    i = ch % 2
                    gi = 10 if ch < 12 else 11
                    P.add("act", lambda e: e.activation(out=qr[:, i, 0:N], in_=ps_ap, func=AF.Copy), reads=[pk], writes=[kqr + str(i)])
                    P.add("act", lambda e: e.activation(out=qsq[:, i, 0:N], in_=ps_ap, func=AF.Square), reads=[pk], writes=[kqsq + str(i)])
                    pb, pk2 = self.ssq([(0, qsq[:, i, 0:N])], N, [kqsq + str(i)])
                    tp, tk = tmps[i]
                    self.rstd_from(pb, pk2, N, 128 * EPS, tp[:, 0:N], tk)
                    src = qr[:, i, 0:N]
                    P.add("dve", lambda e: e.scalar_tensor_tensor(out=src, in0=src, scalar=g[:, gi:gi + 1], in1=tp[:, 0:N], op0=ALU.mult, op1=ALU.mult),
                          reads=[kqr + str(i), tk, "gsc"], writes=[kqr + str(i)])
                    pb3, pk3 = self.psum()
                    P.add("pe", lambda e: e.matmul(pb3[:, 0:N], lhsT=self.matsf[:, 1, :], rhs=src, start=True, stop=True), reads=[kqr + str(i), "matsf"], writes=[pk3])
                    P.add("dve", lambda e: e.tensor_tensor(out=tp[:, 0:N], in0=pb3[:, 0:N], in1=sinT[:, pos0:pos0 + N], op=ALU.mult), reads=[pk3, "rope", tk], writes=[tk])
                    P.add("dve", lambda e: e.tensor_tensor(out=src, in0=src, in1=cosT[:, pos0:pos0 + N], op=ALU.mult), reads=[kqr + str(i), "rope", pk3], writes=[kqr + str(i)])
                    P.add("dve", lambda e: e.tensor_tensor(out=qo[:, i, 0:N], in0=src, in1=tp[:, 0:N], op=ALU.add), reads=[kqr + str(i), tk], writes=[kqo + str(i)])
                    dst = self.qT[ch * 128:(ch + 1) * 128, col0:col0 + N] if ch < 12 else self.kT[(ch - 12) * 128:(ch - 11) * 128, col0:col0 + N]
                    self.dma(dst, qo[:, i, 0:N], [kqo + str(i)], ["qT" if ch < 12 else "kT"])
                else:
                    i = ch % 2
                    P.add("act", lambda e: e.activation(out=qo[:, i, 0:N], in_=ps_ap, func=AF.Copy), reads=[pk], writes=[kqo + str(i)])
                    self.dma(self.fT[(ch - 20) * 128:(ch - 19) * 128, col0:col0 + N], qo[:, i, 0:N], [kqo + str(i)], ["fT"])
            rhs = lambda kc, N=N: u[:, kc, 0:N]

            def evgrp(items, N=N, col0=col0, pos0=pos0):
                for i, (ch, ps_ap, pk) in enumerate(items):
                    P.add("act", lambda e, i=i, ps_ap=ps_ap: e.activation(out=qr[:, i, 0:N], in_=ps_ap, func=AF.Copy), reads=[pk], writes=[kqr + str(i)])
                    P.add("act", lambda e, i=i, ps_ap=ps_ap: e.activation(out=qsq[:, i, 0:N], in_=ps_ap, func=AF.Square), reads=[pk], writes=[kqsq + str(i)])
                pbs = [self.ssq([(0, qsq[:, i, 0:N])], N, [kqsq + str(i)]) for i in range(len(items))]
                for i in range(len(items)):
                    tp, tk = rts[i]
                    P.add("dve", lambda e, i=i, tp=tp: e.tensor_scalar(out=tp[:, 0:N], in0=pbs[i][0][:, 0:N], scalar1=float(128 * EPS), scalar2=None, op0=ALU.add), reads=[pbs[i][1]], writes=[tk])
                for i in range(len(items)):
                    tp, tk = rts[i]
                    P.add("act", lambda e, tp=tp: e.activation(out=tp[:, 0:N], in_=tp[:, 0:N], func=AF.Ln), reads=[tk], writes=[tk])
                for i in range(len(items)):
                    tp, tk = rts[i]
                    P.add("act", lambda e, tp=tp: e.activation(out=tp[:, 0:N], in_=tp[:, 0:N], func=AF.Exp, scale=-0.5), reads=[tk], writes=[tk])
                for i, (ch, ps_ap, pk) in enumerate(items):
                    tp, tk = rts[i]
                    gi = 10 if ch < 12 else 11
                    P.add("dve", lambda e, i=i, tp=tp, gi=gi: e.scalar_tensor_tensor(out=qr[:, i, 0:N], in0=qr[:, i, 0:N], scalar=g[:, gi:gi + 1], in1=tp[:, 0:N], op0=ALU.mult, op1=ALU.mult),
                          reads=[kqr + str(i), tk, "gsc"], writes=[kqr + str(i)])
                pb3 = []
                for i in range(len(items)):
                    p3, k3 = self.psum()
                    pb3.append((p3, k3))
                    P.add("pe", lambda e, i=i, p3=p3: e.matmul(p3[:, 0:N], lhsT=self.matsf[:, 1, :], rhs=qr[:, i, 0:N], start=True, stop=True), reads=[kqr + str(i), "matsf"], writes=[k3])
                for i in range(len(items)):
                    tp, tk = rts[i]
                    P.add("dve", lambda e, i=i, tp=tp: e.tensor_tensor(out=tp[:, 0:N], in0=pb3[i][0][:, 0:N], in1=sinT[:, pos0:pos0 + N], op=ALU.mult), reads=[pb3[i][1], "rope", tk], writes=[tk])
                    P.add("dve", lambda e, i=i: e.tensor_tensor(out=qr[:, i, 0:N], in0=qr[:, i, 0:N], in1=cosT[:, pos0:pos0 + N], op=ALU.mult), reads=[kqr + str(i), "rope", pb3[i][1]], writes=[kqr + str(i)])
                for i, (ch, ps_ap, pk) in enumerate(items):
                    tp, tk = rts[i]
                    P.add("dve", lambda e, i=i, tp=tp: e.tensor_tensor(out=qo[:, i, 0:N], in0=qr[:, i, 0:N], in1=tp[:, 0:N], op=ALU.add), reads=[kqr + str(i), tk], writes=[kqo + str(i)])
                    dst = self.qT[ch * 128:(ch + 1) * 128, col0:col0 + N] if ch < 12 else self.kT[(ch - 12) * 128:(ch - 11) * 128, col0:col0 + N]
                    self.dma(dst, qo[:, i, 0:N], [kqo + str(i)], ["qT" if ch < 12 else "kT"])
            if not is_ctx:
                self.gemm_fm("win1", c.D, 0, 2048, rhs, ukeys, N, evac, evac_group=evgrp)
                self.gemm_fm("win1", c.D, 2560, 3072, rhs, ukeys, N, evac)
            else:
                self.gemm_fm("win1", c.D, 1536, 2048, rhs, ukeys, N, evac, evac_group=evgrp)

            def evv(g0, t0, tn, ps_ap, pk, gc, col0=col0):
                ti = t0 // 128
                P.add("act", lambda e: e.activation(out=vb[0:tn, ti, 0:gc], in_=ps_ap, func=AF.Copy), reads=[pk], writes=[kvb + str(ti)])
                self.dma(self.Vt[col0 + t0:col0 + t0 + tn, 0:512], vb[0:tn, ti, :], [kvb + str(ti)], ["Vt"])
            self.gemm_tm("win1", c.D, 2048, 2560, u, ukeys, N, evv)
        P.flush()

    def stage_L1B(self, merged=False):
        c = self.cfg
        P, A = self.P, self.A
        if not merged:
            A.reset()
            self.slabs = None
        self.cast_some(3)
        NKC = c.TOKB // 128
        NQ = c.S // 128
        kn, kkn = A.alloc((c.TOKB,), BF16)
        vh, kvh = A.alloc((NKC, 128), BF16)
        qh, kqh = A.alloc((c.S,), BF16)
        pts = [A.alloc((128,), BF16) for _ in range(4)]
        wm, _ = A.alloc((2, 128), BF16)
        esink, _ = A.alloc((12,), F32)
        rden, krden = A.alloc((128,), F32)
        ob, kob = A.alloc((c.S,), BF16)
        self.dma(wm, self.wmask_in, [], ["wm"])
        P.add("act", lambda e: e.activation(out=esink, in_=self.vec("sink", 0, 12), func=AF.Exp), reads=["vecs"], writes=["esink"])
        self.ps_lo = 2
        self.psn = 2
        pO, kO, pD, kD = self.ps[0], "ps0", self.ps[1], "ps1"
        for b in range(c.NB):
            cb = b * c.TOKB
            for n in range(4):
                self.dma(kn, self.kT[n * 128:(n + 1) * 128, cb:cb + c.TOKB], ["kT"], [kkn])
                self.dma(vh, self.Vt[cb:cb + c.TOKB, n * 128:(n + 1) * 128].rearrange("(kc p) d -> p kc d", p=128), ["Vt"], [kvh])
                for gq in range(3):
                    yield
                    hq = n * 3 + gq
                    self.dma(qh, self.qT[hq * 128:(hq + 1) * 128, cb:cb + c.S], ["qT"], [kqh])
                    for j in range(NQ):
                        kcs = []
                        if j > 0:
                            kcs.append((j - 1, 0))
                        kcs.append((j, None))
                        if j < NQ - 1:
                            kcs.append((j + 1, 1))
                        kcs += [(kc, None) for kc in range(NQ, NKC)]
                        pend = []
                        for i, (kc, mk) in enumerate(kcs):
                            pS, kS = self.psum()
                            pt, kpt = pts[i % 4]
                            P.add("pe", lambda e, pS=pS, kc=kc, j=j: e.matmul(pS[:, 0:128], lhsT=kn[:, kc * 128:(kc + 1) * 128], rhs=qh[:, j * 128:(j + 1) * 128], start=True, stop=True),
                                  reads=[kkn, kqh], writes=[kS])
                            P.add("act", lambda e, pS=pS, pt=pt: e.activation(out=pt, in_=pS[:, 0:128], func=AF.Exp), reads=[kS], writes=[kpt])
                            if mk is not None:
                                P.add("dve", lambda e, pt=pt, mk=mk: e.tensor_tensor(out=pt, in0=pt, in1=wm[:, mk, :], op=ALU.mult), reads=[kpt, "wm"], writes=[kpt])

                            def mmO(e, pt=pt, kc=kc, first=(i == 0), last=(i == len(kcs) - 1)):
                                e.matmul(pO[:, 0:128], lhsT=vh[:, kc, :], rhs=pt, start=first, stop=last)
                                return e.matmul(pD[:, 0:128], lhsT=self.mats[:, 0, :], rhs=pt, start=first, stop=last)
                            pend.append((mmO, kpt))
                            if len(pend) > 2:
                                fn, kp = pend.pop(0)
                                P.add("pe", fn, reads=[kvh, kp, "mats"], writes=[kO, kD])
                        for fn, kp in pend:
                            P.add("pe", fn, reads=[kvh, kp, "mats"], writes=[kO, kD])
                        P.add("dve", lambda e, hq=hq: e.tensor_scalar(out=rden, in0=pD[:, 0:128], scalar1=esink[:, hq:hq + 1], scalar2=None, op0=ALU.add), reads=[kD, "esink"], writes=[krden])
                        P.add("dve", lambda e: e.reciprocal(out=rden, in_=rden), reads=[krden], writes=[krden])
                        P.add("dve", lambda e, j=j: e.tensor_tensor(out=ob[:, j * 128:(j + 1) * 128], in0=pO[:, 0:128], in1=rden, op=ALU.mult), reads=[kO, krden], writes=[kob])
                    self.dma(self.mixT[hq * 128:(hq + 1) * 128, cb:cb + c.S], ob, [kob], ["mixT"])
        if not merged:
            self.ps_lo = 0
            P.flush()
        yield

    def stage_L1C(self, merged=False):
        c = self.cfg
        P, A = self.P, self.A
        if not merged:
            A.reset()
            self.slabs = None
        self.cast_some(3)
        S = c.S
        NT = S // 128
        NBK = min(512, S)
        fin, kfin = A.alloc((S,), BF16)
        dc, _ = A.alloc((256,), BF16)
        ab, kab = A.alloc((NT, 256), BF16)
        cs = [A.alloc((NT, NBK), BF16) for _ in range(2)]
        yo, kyo = A.alloc((NBK,), BF16)
        self.dma(dc, self.dftc_in, [], ["dc"])
        scale = float(1.0 / np.sqrt(S * 128.0))
        for b in range(c.NB):
            cb = b * c.TOKB
            for gi in range(4):
                yield
                self.dma(fin, self.fT[gi * 128:(gi + 1) * 128, cb:cb + S], ["fT"], [kfin])
                for tt in range(NT):
                    pb, pk = self.psum()
                    P.add("pe", lambda e, pb=pb, tt=tt: e.matmul(pb[:, 0:256], lhsT=fin[:, tt * 128:(tt + 1) * 128], rhs=dc, start=True, stop=True), reads=[kfin, "dc"], writes=[pk])
                    P.add("act", lambda e, pb=pb, tt=tt: e.activation(out=ab[:, tt, :], in_=pb[:, 0:256], func=AF.Copy), reads=[pk], writes=[kab])
                for t0 in range(0, S, NBK):
                    yield
                    for ci in range(2):
                        self.dma(cs[ci][0], self.dftT_in[ci][:, t0:t0 + NBK].rearrange("(tt p) n -> p tt n", p=128), [], [cs[ci][1]])
                    pb, pk = self.psum()

                    def mm(e, pb=pb):
                        m = None
                        for tt in range(NT):
                            e.matmul(pb[:, 0:NBK], lhsT=ab[:, tt, 0:128], rhs=cs[0][0][:, tt, :], start=(tt == 0), stop=False)
                            m = e.matmul(pb[:, 0:NBK], lhsT=ab[:, tt, 128:256], rhs=cs[1][0][:, tt, :], start=False, stop=(tt == NT - 1))
                        return m
                    P.add("pe", mm, reads=[kab, cs[0][1], cs[1][1]], writes=[pk])
                    P.add("act", lambda e, pb=pb: e.activation(out=yo, in_=pb[:, 0:NBK], func=AF.Copy, scale=scale), reads=[pk], writes=[kyo])
                    self.dma(self.mixT[1536 + gi * 128:1536 + (gi + 1) * 128, cb + t0:cb + t0 + NBK], yo, [kyo], ["mixT"])
        if not merged:
            P.flush()
        yield


def run_merged(B, f1, f2):
    B.A.reset()
    B.slabs = None
    gens = [f1(merged=True), f2(merged=True)]
    while gens:
        for g in list(gens):
            try:
                next(g)
            except StopIteration:
                gens.remove(g)
    B.ps_lo = 0
    B.P.flush()


def build_all(cfg):
    B = Builder(cfg, layers=(0, 1))
    B.stage_prep()
    B.stage_mod(0)
    B.stage_L0A()
    run_merged(B, B.stage_L0B, B.stage_L0C)
    B.stage_D(0)
    B.stage_mod(1)
    B.stage_L1A()
    run_merged(B, B.stage_L1B, B.stage_L1C)
    B.stage_D(1)
    B.P.final_wait()
    return B

BF = ml_dtypes.bfloat16

def chunks(v):
    return np.ascontiguousarray(v.reshape(-1, 128).T)

def rope_tables(S, L, rot_dim, rep):
    rows = S // 64
    row = np.repeat(np.arange(rows, dtype=np.float32), 64)
    col = np.tile(np.arange(64, dtype=np.float32), rows)
    half = rot_dim // 2
    inv = (10000.0 ** (-np.arange(0, half, 2, dtype=np.float32) / half)).astype(np.float32)
    nf = len(inv)
    ang_r = row[:, None] * inv[None, :]
    ang_c = col[:, None] * inv[None, :]
    cos = np.concatenate([np.cos(ang_r), np.cos(ang_r), np.cos(ang_c), np.cos(ang_c)], 1).T
    sin = np.concatenate([np.sin(ang_r), np.sin(ang_r), np.sin(ang_c), np.sin(ang_c)], 1).T
    cos = np.concatenate([cos, np.ones((rot_dim, L), np.float32)], 1)
    sin = np.concatenate([sin, np.zeros((rot_dim, L), np.float32)], 1)
    return np.tile(cos, (rep, 1)).astype(np.float32), np.tile(sin, (rep, 1)).astype(np.float32)

def perm_matrix(rot_dim, rep):
    nf = rot_dim // 4
    Pm = np.zeros((rot_dim, rot_dim), np.float32)
    for base in (0, 2 * nf):
        for i in range(nf):
            Pm[base + i, base + nf + i] = -1.0
            Pm[base + nf + i, base + i] = 1.0
    full = np.zeros((128, 128), np.float32)
    for r in range(rep):
        full[r * rot_dim:(r + 1) * rot_dim, r * rot_dim:(r + 1) * rot_dim] = Pm
    return np.ascontiguousarray(full.T)

def prep_common(cfg, inp):
    D = cfg.D
    out = {}
    w0 = inp["even_w_in"][0]
    out["w_win0"] = np.ascontiguousarray(np.concatenate([w0[:, 0:768], w0[:, 832:2880], w0[:, 768:832], w0[:, 768:832]], 1))
    wq = inp["mla_w_q_b"][0].reshape(512, 8, 192)
    out["w_wqb"] = np.ascontiguousarray(np.concatenate([wq[:, :, :128].reshape(512, 1024), wq[:, :, 128:].reshape(512, 512)], 1))
    wk = inp["mla_w_kv_b"][0].reshape(256, 8, 256)
    out["w_wkvb"] = np.ascontiguousarray(np.concatenate([wk[:, :, :128].reshape(256, 1024), wk[:, :, 128:].reshape(256, 1024)], 1))
    out["w_ada0"] = inp["ada_w"][0]; out["w_ada1"] = inp["ada_w"][1]
    out["w_wout0"] = inp["even_w_out"][0]
    out["w_dg"] = inp["dense_w_gate"][0]; out["w_du"] = inp["dense_w_up"][0]; out["w_dd"] = inp["dense_w_down"][0]
    out["w_win1"] = inp["odd_w_in"][0]; out["w_wout1"] = inp["odd_w_out"][0]
    for e in range(cfg.E):
        out["w_eg%d" % e] = inp["expert_w_gate"][0, e]; out["w_eu%d" % e] = inp["expert_w_up"][0, e]; out["w_ed%d" % e] = inp["expert_w_down"][0, e]
    off, nv = vec_layout(cfg)
    V = np.zeros((128, nv), np.float32)
    def put(name, arr):
        o, w = off[name]; assert arr.shape == (128, w), (name, arr.shape, w); V[:, o:o + w] = arr
    put("mixg0", chunks(inp["mix_norm_g"][0])); put("mixg1", chunks(inp["mix_norm_g"][1]))
    put("ffng0", chunks(inp["ffn_norm_g"][0])); put("ffng1", chunks(inp["ffn_norm_g"][1]))
    put("adab0", chunks(inp["ada_b"][0])); put("adab1", chunks(inp["ada_b"][1]))
    put("qag", chunks(inp["mla_q_a_norm_g"][0])); put("kvag", chunks(inp["mla_kv_a_norm_g"][0]))
    qg = inp["mla_q_norm_g"][0]; kg = inp["mla_k_norm_g"][0]
    put("qng_n", qg[:128, None]); put("qng_r", np.tile(qg[128:], 2)[:, None])
    put("kng_n", kg[:128, None]); put("kng_r", np.tile(kg[128:], 2)[:, None])
    dw = inp["conv_dw_w"][0]
    put("dww", np.ascontiguousarray(dw.T.reshape(8, 128, 31).transpose(1, 0, 2)).reshape(128, 8 * 31))
    put("dwb", chunks(inp["conv_dw_b"][0])); put("lng", chunks(inp["conv_ln_g"][0])); put("lnb", chunks(inp["conv_ln_b"][0]))
    put("sqg", inp["swa_q_norm_g"][0][:, None]); put("skg", inp["swa_k_norm_g"][0][:, None])
    put("sink", np.tile(inp["swa_sink"][0][None, :], (128, 1)))
    rw = inp["router_w"][0]
    put("rw", np.ascontiguousarray(rw.reshape(cfg.KC, 128, 8).transpose(1, 0, 2)).reshape(128, cfg.KC * 8))
    out["vecs"] = V
    M = np.zeros((128, 6, 128), np.float32)
    M[:, 0, :] = 1.0
    M[0:64, 1, :] = 1.0
    M[64:128, 2, :] = 1.0
    M[:, 3, 0:64] = 1.0
    M[:, 4, 64:128] = 1.0
    M[0:64, 5, 0:64] = 1.0; M[64:128, 5, 64:128] = 1.0
    out["mats"] = M.astype(BF)
    MF = np.zeros((128, 4, 128), np.float32)
    MF[:, 0, :] = perm_matrix(64, 2)
    MF[:, 1, :] = perm_matrix(128, 1)
    MF[:, 2, :] = np.eye(128, dtype=np.float32)
    MF[:, 3, :] = 1.0
    out["matsf"] = MF
    c0, s0 = rope_tables(cfg.S, cfg.L, 64, 2)
    c1, s1 = rope_tables(cfg.S, cfg.L, 128, 1)
    out["rope"] = np.ascontiguousarray(np.stack([c0, s0, c1, s1], 1))
    wm = np.zeros((128, 2, 128), np.float32)
    kk = np.arange(128)[:, None]; qq = np.arange(128)[None, :]
    wm[:, 0, :] = (qq <= kk); wm[:, 1, :] = (kk <= qq)
    out["wmask"] = wm.astype(BF)
    sel = np.zeros((8, 8, 128), np.float32)
    for e_ in range(8): sel[e_, e_, :] = 1.0
    out["selm"] = sel
    cc = np.arange(128)
    ang = 2 * np.pi * np.outer(cc, cc) / 128.0
    out["dftc"] = np.concatenate([np.cos(ang), -np.sin(ang)], 1).astype(np.float32).astype(BF)
    tt = np.arange(cfg.S, dtype=np.int64)
    angT = 2 * np.pi * ((np.outer(tt, tt) % cfg.S).astype(np.float64)) / cfg.S
    out["dftT"] = np.stack([np.cos(angT), np.sin(angT)], 0).astype(np.float32).astype(BF)
    return out

def prep_core(cfg, inp, core):
    NB = cfg.NB
    bs = slice(core * NB, (core + 1) * NB)
    o = {}
    o["xT"] = np.ascontiguousarray(inp["x"][bs].transpose(0, 2, 1))
    o["cT"] = np.ascontiguousarray(inp["ctx"][bs].transpose(0, 2, 1))
    o["cvec"] = np.ascontiguousarray(np.concatenate([inp["c"][bs], inp["c_ctx"][None, :]], 0).T)
    return o


def kernel(**inputs):
    from concourse.bass_utils import run_bass_kernel_spmd
    inp = {k: np.asarray(v) for k, v in inputs.items()}
    cfg = Cfg(NB=2, S=2048, L=256, D=2048, DFF=5632, EFF=7168, E=8)
    NCORES = 8
    B = build_all(cfg)
    common = prep_common(cfg, inp)
    in_maps = []
    for c in range(NCORES):
        m = dict(common)
        m.update(prep_core(cfg, inp, c))
        in_maps.append(m)
    res = run_bass_kernel_spmd(B.nc, in_maps, core_ids=list(range(NCORES)))
    outs = [np.ascontiguousarray(np.asarray(r["outT"]).transpose(0, 2, 1)) for r in res.results]
    return np.concatenate(outs, axis=0).astype(np.float32)
```
